# Optimizing a Trainium2 kernel written in Bass

```python
import math
import jax, jax.numpy as jnp
from jax import lax
import numpy as np

D_MODEL = 2048
BATCH = 4
SEQ = 2048
DEPTH = 4

N_META = 16
EPS = 1e-6
ATT_HEADS = 8
QK_NOPE = 128
QK_ROPE = 64
V_DIM = 128
Q_LORA = 512
KV_LORA = 256
ROPE_THETA = 10000.0
Q_BLOCK = 128
ATT_WIDTH = ATT_HEADS * V_DIM
SSM_GROUP = 16
SSM_WIDTH = D_MODEL - ATT_WIDTH
SSM_GROUPS = SSM_WIDTH // SSM_GROUP
SSM_STATE = 64
DT_MIN = 1e-3
DT_MAX = 1e-1
MIX_WIDTH = ATT_WIDTH + SSM_WIDTH
IN_COLS = Q_LORA + KV_LORA + QK_ROPE + SSM_WIDTH
D_FF = 5632
N_EXPERTS = 8
TOP_K = 2
D_FF_EXPERT = 1408
N_DENSE = (DEPTH + 1) // 2
N_MOE = DEPTH // 2

kernel_name = "hybrid_mla_s5_moe_encoder"

F32 = jnp.float32


def rms_norm(x, g):
    xf = x.astype(F32)
    y = xf * lax.rsqrt(jnp.mean(xf * xf, axis=-1, keepdims=True) + EPS)
    return (y * g.astype(F32)).astype(x.dtype)


def rope_tables(n):
    inv = ROPE_THETA ** (-jnp.arange(0, QK_ROPE, 2, dtype=F32) / QK_ROPE)
    ang = jnp.arange(n, dtype=F32)[:, None] * inv[None, :]
    return jnp.cos(ang), jnp.sin(ang)


def apply_rope(x, cos, sin):
    xf = x.astype(F32)
    x1, x2 = jnp.split(xf, 2, axis=-1)
    return jnp.concatenate([x1 * cos - x2 * sin, x1 * sin + x2 * cos], axis=-1).astype(x.dtype)


def mla_attention(c_q, c_kv, k_rope, q_norm, w_uq, kv_norm, w_ukv, cos, sin):
    bsz, L, _ = c_q.shape
    q = (rms_norm(c_q, q_norm) @ w_uq).reshape(bsz, L, ATT_HEADS, QK_NOPE + QK_ROPE)
    q = jnp.concatenate([q[..., :QK_NOPE], apply_rope(q[..., QK_NOPE:], cos[:, None, :], sin[:, None, :])], axis=-1)
    kv = (rms_norm(c_kv, kv_norm) @ w_ukv).reshape(bsz, L, ATT_HEADS, QK_NOPE + V_DIM)
    k_nope, v = kv[..., :QK_NOPE], kv[..., QK_NOPE:]
    k_pe = apply_rope(k_rope, cos, sin)
    k = jnp.concatenate([k_nope, jnp.broadcast_to(k_pe[:, :, None, :], (bsz, L, ATT_HEADS, QK_ROPE))], axis=-1)
    n_blk = -(-L // Q_BLOCK)
    lp = n_blk * Q_BLOCK
    q = jnp.pad(q, ((0, 0), (0, lp - L), (0, 0), (0, 0)))
    qb = q.reshape(bsz, n_blk, Q_BLOCK, ATT_HEADS, QK_NOPE + QK_ROPE).transpose(1, 0, 2, 3, 4)
    scale = (QK_NOPE + QK_ROPE) ** -0.5

    def block(q_blk):
        s = jnp.einsum('bqhd,bkhd->bhqk', q_blk, k, preferred_element_type=F32) * scale
        p = jax.nn.softmax(s, axis=-1)
        return jnp.einsum('bhqk,bkhd->bqhd', p.astype(v.dtype), v)

    o = lax.map(block, qb)
    return o.transpose(1, 0, 2, 3, 4).reshape(bsz, lp, ATT_WIDTH)[:, :L]


def s5_direction(u_c, lam_re, lam_im, log_step, b_re, b_im, c_re, c_im, reverse):
    lam = lax.complex(lam_re.astype(F32), lam_im.astype(F32))
    dt = jnp.exp(log_step.astype(F32))[:, None]
    lam_bar = jnp.exp(lam * dt)
    b = lax.complex(b_re.astype(F32), b_im.astype(F32))
    b_bar = ((lam_bar - 1.0) / lam)[..., None] * b
    bu = jnp.einsum('gpc,blgc->blgp', b_bar, u_c)
    a = jnp.broadcast_to(lam_bar, bu.shape)

    def combine(left, right):
        a_l, b_l = left
        a_r, b_r = right
        return a_r * a_l, a_r * b_l + b_r

    _, h = lax.associative_scan(combine, (a, bu), axis=1, reverse=reverse)
    c = lax.complex(c_re.astype(F32), c_im.astype(F32))
    return jnp.real(jnp.einsum('gcp,blgp->blgc', c, h))


def s5_mixer(u, lam_re, lam_im, log_step, b_re, b_im, c_re, c_im, d_skip, w_glu):
    bsz, L, _ = u.shape
    uf = u.astype(F32)
    ug = uf.reshape(bsz, L, SSM_GROUPS, SSM_GROUP)
    uc = lax.complex(ug, jnp.zeros_like(ug))
    y_fwd = s5_direction(uc, lam_re[0], lam_im[0], log_step[0], b_re[0], b_im[0], c_re[0], c_im[0], False)
    y_bwd = s5_direction(uc, lam_re[1], lam_im[1], log_step[1], b_re[1], b_im[1], c_re[1], c_im[1], True)
    y = (y_fwd + y_bwd).reshape(bsz, L, SSM_WIDTH) + d_skip.astype(F32) * uf
    g = jax.nn.gelu(y).astype(u.dtype)
    return g * jax.nn.sigmoid(g @ w_glu)


def swiglu(x, w_gate, w_up, w_down):
    return (jax.nn.silu(x @ w_gate) * (x @ w_up)) @ w_down


def moe_swiglu(x, w_router, w_gate, w_up, w_down):
    bsz, L, d = x.shape
    t = x.reshape(bsz * L, d)
    logits = (t @ w_router).astype(F32)
    top_val, top_idx = lax.top_k(logits, TOP_K)
    top_w = jax.nn.softmax(top_val, axis=-1)
    gates = jnp.sum(jax.nn.one_hot(top_idx, N_EXPERTS, dtype=F32) * top_w[..., None], axis=1)
    hid = jax.nn.silu(jnp.einsum('td,edf->etf', t, w_gate)) * jnp.einsum('td,edf->etf', t, w_up)
    hid = hid * gates.T[:, :, None].astype(hid.dtype)
    out = jnp.einsum('etf,efd->td', hid, w_down)
    return out.reshape(bsz, L, d)


def setup_inputs(seed: int = 0) -> dict:
    key = jax.random.key(seed)
    ks = iter(jax.random.split(key, 40))
    nrm = lambda shape, s: jax.random.normal(next(ks), shape, F32) * s
    gain = lambda shape: 1.0 + 0.01 * jax.random.normal(next(ks), shape, F32)
    res_scale = (2 * DEPTH) ** -0.5
    n_idx = jnp.arange(SSM_STATE, dtype=F32)
    lam_re = -0.5 + 0.01 * jax.random.normal(next(ks), (DEPTH, 2, SSM_GROUPS, SSM_STATE), F32)
    lam_im = math.pi * n_idx + 0.01 * jax.random.normal(next(ks), (DEPTH, 2, SSM_GROUPS, SSM_STATE), F32)
    log_step = jax.random.uniform(next(ks), (DEPTH, 2, SSM_GROUPS), F32, math.log(DT_MIN), math.log(DT_MAX))
    return {
        "x": nrm((BATCH, SEQ, D_MODEL), 1.0),
        "meta_tokens": nrm((N_META, D_MODEL), 1.0),
        "mix_norm": gain((DEPTH, D_MODEL)),
        "w_in": nrm((DEPTH, D_MODEL, IN_COLS), D_MODEL ** -0.5),
        "q_norm": gain((DEPTH, Q_LORA)),
        "w_uq": nrm((DEPTH, Q_LORA, ATT_HEADS * (QK_NOPE + QK_ROPE)), Q_LORA ** -0.5),
        "kv_norm": gain((DEPTH, KV_LORA)),
        "w_ukv": nrm((DEPTH, KV_LORA, ATT_HEADS * (QK_NOPE + V_DIM)), KV_LORA ** -0.5),
        "ssm_lambda_re": lam_re,
        "ssm_lambda_im": lam_im,
        "ssm_log_step": log_step,
        "ssm_b_re": nrm((DEPTH, 2, SSM_GROUPS, SSM_STATE, SSM_GROUP), (2 * SSM_GROUP) ** -0.5),
        "ssm_b_im": nrm((DEPTH, 2, SSM_GROUPS, SSM_STATE, SSM_GROUP), (2 * SSM_GROUP) ** -0.5),
        "ssm_c_re": nrm((DEPTH, 2, SSM_GROUPS, SSM_GROUP, SSM_STATE), 0.5),
        "ssm_c_im": nrm((DEPTH, 2, SSM_GROUPS, SSM_GROUP, SSM_STATE), 0.5),
        "ssm_d": nrm((DEPTH, SSM_WIDTH), 0.5),
        "ssm_w_glu": nrm((DEPTH, SSM_WIDTH, SSM_WIDTH), SSM_WIDTH ** -0.5),
        "attn_out_norm": gain((DEPTH, ATT_WIDTH)),
        "ssm_out_norm": gain((DEPTH, SSM_WIDTH)),
        "w_out": nrm((DEPTH, MIX_WIDTH, D_MODEL), MIX_WIDTH ** -0.5 * res_scale),
        "ffn_norm": gain((DEPTH, D_MODEL)),
        "dense_w_gate": nrm((N_DENSE, D_MODEL, D_FF), D_MODEL ** -0.5),
        "dense_w_up": nrm((N_DENSE, D_MODEL, D_FF), D_MODEL ** -0.5),
        "dense_w_down": nrm((N_DENSE, D_FF, D_MODEL), D_FF ** -0.5 * res_scale),
        "moe_router": nrm((N_MOE, D_MODEL, N_EXPERTS), D_MODEL ** -0.5),
        "moe_w_gate": nrm((N_MOE, N_EXPERTS, D_MODEL, D_FF_EXPERT), D_MODEL ** -0.5),
        "moe_w_up": nrm((N_MOE, N_EXPERTS, D_MODEL, D_FF_EXPERT), D_MODEL ** -0.5),
        "moe_w_down": nrm((N_MOE, N_EXPERTS, D_FF_EXPERT, D_MODEL), D_FF_EXPERT ** -0.5 * res_scale),
        "final_norm": gain((D_MODEL,)),
    }


def reference(x, meta_tokens, mix_norm, w_in, q_norm, w_uq, kv_norm, w_ukv,
              ssm_lambda_re, ssm_lambda_im, ssm_log_step, ssm_b_re, ssm_b_im, ssm_c_re, ssm_c_im,
              ssm_d, ssm_w_glu, attn_out_norm, ssm_out_norm, w_out, ffn_norm,
              dense_w_gate, dense_w_up, dense_w_down,
              moe_router, moe_w_gate, moe_w_up, moe_w_down, final_norm):
    bsz, seq, d = x.shape
    L = N_META + seq
    meta = jnp.broadcast_to(meta_tokens[None].astype(x.dtype), (bsz, N_META, d))
    h = jnp.concatenate([meta, x], axis=1)
    cos, sin = rope_tables(L)
    splits = [Q_LORA, Q_LORA + KV_LORA, Q_LORA + KV_LORA + QK_ROPE]
    for layer in range(DEPTH):
        hn = rms_norm(h, mix_norm[layer])
        proj = hn @ w_in[layer]
        c_q, c_kv, k_rope, u = jnp.split(proj, splits, axis=-1)
        att = mla_attention(c_q, c_kv, k_rope, q_norm[layer], w_uq[layer],
                            kv_norm[layer], w_ukv[layer], cos, sin)
        ssm = s5_mixer(u, ssm_lambda_re[layer], ssm_lambda_im[layer], ssm_log_step[layer],
                       ssm_b_re[layer], ssm_b_im[layer], ssm_c_re[layer], ssm_c_im[layer],
                       ssm_d[layer], ssm_w_glu[layer])
        mixed = jnp.concatenate([rms_norm(att, attn_out_norm[layer]),
                                 rms_norm(ssm.astype(h.dtype), ssm_out_norm[layer])], axis=-1)
        h = h + mixed @ w_out[layer]
        hn = rms_norm(h, ffn_norm[layer])
        if layer % 2 == 0:
            i = layer // 2
            h = h + swiglu(hn, dense_w_gate[i], dense_w_up[i], dense_w_down[i])
        else:
            i = layer // 2
            h = h + moe_swiglu(hn, moe_router[i], moe_w_gate[i], moe_w_up[i], moe_w_down[i])
    out = rms_norm(h, final_norm)[:, N_META:]
    return out
```

```python
import math
from contextlib import ExitStack

import numpy as np
import ml_dtypes

import concourse.bass as bass
import concourse.mybir as mybir
from concourse.bass_utils import run_bass_kernel_spmd

F32 = mybir.dt.float32
BF16 = mybir.dt.bfloat16
I32 = mybir.dt.int32
I16 = mybir.dt.int16
AF = mybir.ActivationFunctionType
ALU = mybir.AluOpType
AX = mybir.AxisListType

NCORES = 8
D = 2048
DEPTH = 4
SEQ = 2048
NMETA = 16
LTOT = SEQ + NMETA
T = LTOT // 2
CH = [(0, 344), (344, 344), (688, 344)]
CHL = [(0, 512), (512, 512), (1024, 512), (1536, 512), (2048, 16)]
NKT = 17
EPS = 1e-6
HEADS = 8
NG = 32
NGD = 64
NST = 11
NSL = 2 * NST
TWO_PI_LO = 6.283185


class Tl:
    __slots__ = ("w", "r", "name")

    def __init__(self, name=""):
        self.w = None
        self.r = {}
        self.name = name


class Eng:
    def __init__(self, name, obj):
        self.name = name
        self.obj = obj
        self.semid = None
        self.cnt = 0
        self.seen = {}


class K:
    def __init__(self, nc, es):
        self.nc = nc
        self.es = es
        self.sems = []
        self.engs = {}
        for name, obj in (("pe", nc.tensor), ("dve", nc.vector), ("act", nc.scalar),
                          ("pool", nc.gpsimd), ("sp", nc.sync)):
            e = Eng(name, obj)
            e.semid = self.new_sem("e_" + name)
            self.engs[name] = e
        self.dslots = {}
        for q, n in (("sp", 24), ("pool", 24), ("act", 8)):
            self.dslots[q] = [[self.new_sem("d_%s%d" % (q, i)), 0] for i in range(n)]
        self.dnext = {"sp": 0, "pool": 0, "act": 0}
        self.psum = [es.enter_context(nc.psum_tensor("psb%d" % i, [128, 512], F32)) for i in range(8)]
        self.psum_tl = [Tl("ps%d" % i) for i in range(8)]
        self.ps_next = 0
        self.uid = 0
        self.cc_sem = self.new_sem("cc")
        self.cc_cnt = 0

    def new_sem(self, name):
        s = self.es.enter_context(self.nc.semaphore(name))
        self.sems.append(s)
        return len(self.sems) - 1

    def sb(self, es, name, shape, dt):
        self.uid += 1
        return es.enter_context(self.nc.sbuf_tensor("%s_%d" % (name, self.uid), shape, dt))

    def _wait(self, eng, reads, writes):
        deps = {}
        for t in reads:
            if t.w is not None:
                deps[t.w[0]] = max(deps.get(t.w[0], 0), t.w[1])
        for t in writes:
            if t.w is not None and t.w[0] != eng.semid:
                deps[t.w[0]] = max(deps.get(t.w[0], 0), t.w[1])
            for sid, v in t.r.items():
                if sid != eng.semid:
                    deps[sid] = max(deps.get(sid, 0), v)
        for sid, v in deps.items():
            if sid == eng.semid and eng.name == "pe":
                continue
            if eng.seen.get(sid, 0) >= v:
                continue
            eng.obj.wait_ge(self.sems[sid], v)
            eng.seen[sid] = v

    def _commit(self, tok, reads, writes):
        for t in writes:
            t.w = tok
            t.r = {}
        for t in reads:
            t.r[tok[0]] = max(t.r.get(tok[0], 0), tok[1])

    def op(self, e, reads, writes, fn):
        eng = self.engs[e]
        self._wait(eng, reads, writes)
        inst = fn(eng.obj)
        eng.cnt += 1
        inst.then_inc(self.sems[eng.semid], 1)
        self._commit((eng.semid, eng.cnt), reads, writes)

    def dma(self, q, out, in_, reads, writes, **kw):
        eng = self.engs[q]
        self._wait(eng, reads, writes)
        slots = self.dslots[q]
        i = self.dnext[q]
        self.dnext[q] = (i + 1) % len(slots)
        sid, val = slots[i]
        if val > 0 and eng.seen.get(sid, 0) < val:
            eng.obj.wait_ge(self.sems[sid], val)
            eng.seen[sid] = val
        inst = eng.obj.dma_start(out=out, in_=in_, **kw)
        inst.then_inc(self.sems[sid], 16)
        slots[i][1] = val + 16
        self._commit((sid, val + 16), reads, writes)

    def collective(self, in_t, out_t, in_tls, out_tls, groups):
        eng = self.engs["pool"]
        self._wait(eng, in_tls, out_tls)
        inst = eng.obj.collective_compute("AllGather", ALU.bypass, replica_groups=groups,
                                          ins=[in_t.ap().opt()], outs=[out_t.ap().opt()])
        self.cc_cnt += 1
        inst.then_inc(self.sems[self.cc_sem], 1)
        self._commit((self.cc_sem, self.cc_cnt), in_tls, out_tls)

    def barrier(self):
        sp = self.engs["sp"]
        for n, e in self.engs.items():
            if n != "sp" and e.cnt > 0 and sp.seen.get(e.semid, 0) < e.cnt:
                sp.obj.wait_ge(self.sems[e.semid], e.cnt)
                sp.seen[e.semid] = e.cnt
        for q in self.dslots:
            for sid, val in self.dslots[q]:
                if val > 0 and sp.seen.get(sid, 0) < val:
                    sp.obj.wait_ge(self.sems[sid], val)
                    sp.seen[sid] = val
        if self.cc_cnt > 0 and sp.seen.get(self.cc_sem, 0) < self.cc_cnt:
            sp.obj.wait_ge(self.sems[self.cc_sem], self.cc_cnt)
            sp.seen[self.cc_sem] = self.cc_cnt
        inst = sp.obj.nop()
        sp.cnt += 1
        inst.then_inc(self.sems[sp.semid], 1)
        for n, e in self.engs.items():
            if n != "sp":
                e.obj.wait_ge(self.sems[sp.semid], sp.cnt)
                e.seen[sp.semid] = sp.cnt

    def finish(self):
        self.barrier()

    def ps(self):
        i = self.ps_next
        self.ps_next = (i + 1) % 8
        return self.psum[i], self.psum_tl[i]


class Ctx:
    pass


def make_consts(k, es):
    c = Ctx()
    c.ones_f = k.sb(es, "ones_f", [128, 128], F32)
    c.ones_b = k.sb(es, "ones_b", [128, 128], BF16)
    c.tl = Tl("consts")
    k.op("dve", [], [c.tl], lambda e: e.memset(c.ones_f[:], 1.0))
    k.op("dve", [], [c.tl], lambda e: e.memset(c.ones_b[:], 1.0))
    c.eps = k.sb(es, "eps", [128, 1], F32)
    k.op("dve", [], [c.tl], lambda e: e.memset(c.eps[:], EPS))
    c.offs = k.sb(es, "offs", [128, 2], F32)
    k.op("dve", [], [c.tl], lambda e: e.memset(c.offs[:, 0:1], 0.0))
    k.op("dve", [], [c.tl], lambda e: e.memset(c.offs[:, 1:2], 0.25))
    return c


class WPool:
    def __init__(self, k, es, n, name="wslab", kt=16, mw=128):
        self.slabs = [(k.sb(es, name, [128, kt, mw], BF16), Tl(name)) for _ in range(n)]
        self.i = 0

    def get(self):
        s = self.slabs[self.i]
        self.i = (self.i + 1) % len(self.slabs)
        return s


def linear(k, wpool, w_dram, MT, KT, mw, rhs_fn, chunks, evac, q="pool"):
    for m in range(MT):
        slab, stl = wpool.get()
        k.dma(q, slab[:, 0:KT, 0:mw], w_dram[m], [], [stl])
        for ci, (n0, nsz) in enumerate(chunks):
            ps, pst = k.ps()
            for kt in range(KT):
                rap, rt = rhs_fn(kt, ci)
                k.op("pe", [stl, rt], [pst],
                     lambda e: e.matmul(ps[0:mw, 0:nsz], slab[:, kt, 0:mw], rap,
                                        start=(kt == 0), stop=(kt == KT - 1)))
            evac(m, ci, ps, pst, n0, nsz)


class NormTmp:
    def __init__(self, k, es, width=344):
        self.sq = [(k.sb(es, "sq", [128, width], F32), Tl("sq")) for _ in range(3)]
        self.rs = [(k.sb(es, "rs", [128, width], F32), Tl("rs")) for _ in range(2)]
        self.si = 0
        self.ri = 0


def rmsnorm(k, c, nt, x, x_tl, KT, chunks, gain, gain_tl, out, out_tl, dim, out_off=0, keep=None):
    for ci, (n0, nsz) in enumerate(chunks):
        ps, pst = k.ps()
        for kt in range(KT):
            s, stl = nt.sq[nt.si % 3]
            nt.si += 1
            k.op("act", [x_tl[ci]], [stl],
                 lambda e: e.activation(s[:, 0:nsz], x[:, kt, n0:n0 + nsz], AF.Square))
            k.op("pe", [stl, c.tl], [pst],
                 lambda e: e.matmul(ps[:, 0:nsz], c.ones_f[:], s[:, 0:nsz],
                                    start=(kt == 0), stop=(kt == KT - 1)))
        if keep is None:
            r, rtl = nt.rs[nt.ri % 2]
            nt.ri += 1
            rap = r[:, 0:nsz]
        else:
            r, rtl = keep[0], keep[1][ci]
            rap = r[:, n0:n0 + nsz]
        k.op("act", [pst], [rtl],
             lambda e: e.activation(rap, ps[:, 0:nsz], AF.Sqrt, bias=c.eps[:, 0:1], scale=1.0 / dim))
        k.op("dve", [rtl], [rtl], lambda e: e.reciprocal(rap, rap))
        for kt in range(KT):
            k.op("dve", [x_tl[ci], rtl, gain_tl], [out_tl[ci]],
                 lambda e: e.scalar_tensor_tensor(out[:, out_off + kt, n0:n0 + nsz], x[:, kt, n0:n0 + nsz],
                                                  gain[:, kt:kt + 1], rap, ALU.mult, ALU.mult))


def load_small(k, es, name, dram_ap, shape, dt=F32, q="sp"):
    t = k.sb(es, name, shape, dt)
    tl = Tl(name)
    k.dma(q, t[:], dram_ap, [], [tl])
    return t, tl


def rope_evac(k, psA, psAt, psB, psBt, ropeC, ropeC_tl, ropeS, ropeS_tl, tmpA, tmpB, out_ap, out_tl, n0, nsz):
    t1, t1l = tmpA
    t2, t2l = tmpB
    k.op("dve", [psBt, ropeS_tl], [t1l],
         lambda e: e.tensor_tensor(t1[:, 0:nsz], psB[0:64, 0:nsz], ropeS[:, n0:n0 + nsz], ALU.mult))
    k.op("dve", [psAt, ropeC_tl], [t2l],
         lambda e: e.tensor_tensor(t2[:, 0:nsz], psA[0:64, 0:nsz], ropeC[:, n0:n0 + nsz], ALU.mult))
    k.op("dve", [t1l, t2l], [out_tl],
         lambda e: e.tensor_tensor(out_ap, t1[:, 0:nsz], t2[:, 0:nsz], ALU.add))


def phase_a(k, c, io, h, h_tl):
    with ExitStack() as es:
        g_mix, g_mix_tl = load_small(k, es, "g_mix", io["mix_g"], [128, 16])
        g_q, g_q_tl = load_small(k, es, "g_q", io["q_g"], [128, 4])
        g_kv, g_kv_tl = load_small(k, es, "g_kv", io["kv_g"], [128, 2])
        ropeC, ropeC_tl = load_small(k, es, "ropeC", io["ropeC"], [64, T])
        ropeS, ropeS_tl = load_small(k, es, "ropeS", io["ropeS"], [64, T])
        nt = NormTmp(k, es)
        hn = k.sb(es, "hn", [128, 16, T], BF16)
        hn_tl = [Tl("hn") for _ in CH]
        rmsnorm(k, c, nt, h, h_tl, 16, CH, g_mix, g_mix_tl, hn, hn_tl, D)
        wpool = WPool(k, es, 4)
        pj = k.sb(es, "pj", [128, 4, T], F32)
        pj_tl = [Tl("pj") for _ in CH]
        kpe = k.sb(es, "kpe", [64, T], BF16)
        kpe_tl = [Tl("kpe") for _ in CH]
        stg = [(k.sb(es, "stg", [128, 344], BF16), Tl("stg")) for _ in range(3)]
        tmpA = [(k.sb(es, "rtmp", [64, 344], F32), Tl("rtmp")) for _ in range(2)]
        tmpB = [(k.sb(es, "rtmp2", [64, 344], F32), Tl("rtmp2")) for _ in range(2)]
        cqn = k.sb(es, "cqn", [128, 4, T], BF16)
        cqn_tl = [Tl("cqn") for _ in CH]
        ckvn = k.sb(es, "ckvn", [128, 2, T], BF16)
        ckvn_tl = [Tl("ckvn") for _ in CH]
        w = io["w_in_t"]

        def rhs(kt, ci):
            n0, nsz = CH[ci]
            return hn[:, kt, n0:n0 + nsz], hn_tl[ci]

        def evac_pj(off):
            def f(m, ci, ps, pst, n0, nsz):
                if (m + ci) % 2 == 0:
                    k.op("act", [pst], [pj_tl[ci]], lambda e: e.copy(pj[:, m, n0:n0 + nsz], ps[:, 0:nsz]))
                else:
                    k.op("dve", [pst], [pj_tl[ci]], lambda e: e.tensor_copy(pj[:, m, n0:n0 + nsz], ps[:, 0:nsz]))
            return f

        linear(k, wpool, w[0:4], 4, 16, 128, rhs, CH, evac_pj(0))
        rmsnorm(k, c, nt, pj, pj_tl, 4, CH, g_q, g_q_tl, cqn, cqn_tl, 512)
        for kt in range(4):
            k.dma("sp", io["cq_n"][kt * 128:(kt + 1) * 128, :], cqn[:, kt, :], cqn_tl, io["cq_n_tl"])
        linear(k, wpool, w[4:6], 2, 16, 128, rhs, CH, evac_pj(0))
        rmsnorm(k, c, nt, pj, pj_tl, 2, CH, g_kv, g_kv_tl, ckvn, ckvn_tl, 256)
        for kt in range(2):
            k.dma("sp", io["kvx"][kt * 128:(kt + 1) * 128, :], ckvn[:, kt, :], ckvn_tl, io["kvx_tl"])
        slab, stl = wpool.get()
        k.dma("pool", slab[:, 0:16, :], w[6], [], [stl])
        for ci, (n0, nsz) in enumerate(CH):
            psA, psAt = k.ps()
            psB, psBt = k.ps()
            for kt in range(16):
                k.op("pe", [stl, hn_tl[ci]], [psAt],
                     lambda e: e.matmul(psA[0:64, 0:nsz], slab[:, kt, 0:64], hn[:, kt, n0:n0 + nsz],
                                        start=(kt == 0), stop=(kt == 15)))
            for kt in range(16):
                k.op("pe", [stl, hn_tl[ci]], [psBt],
                     lambda e: e.matmul(psB[0:64, 0:nsz], slab[:, kt, 64:128], hn[:, kt, n0:n0 + nsz],
                                        start=(kt == 0), stop=(kt == 15)))
            rope_evac(k, psA, psAt, psB, psBt, ropeC, ropeC_tl, ropeS, ropeS_tl, tmpA[ci % 2], tmpB[ci % 2],
                      kpe[:, n0:n0 + nsz], kpe_tl[ci], n0, nsz)
        k.dma("sp", io["kvx"][256:320, :], kpe[:, :], kpe_tl, io["kvx_tl"])
        cnt = [0]

        def evac_u(m, ci, ps, pst, n0, nsz):
            s, sl = stg[cnt[0] % 3]
            cnt[0] += 1
            if cnt[0] % 2 == 0:
                k.op("act", [pst], [sl], lambda e: e.copy(s[:, 0:nsz], ps[:, 0:nsz]))
            else:
                k.op("dve", [pst], [sl], lambda e: e.tensor_copy(s[:, 0:nsz], ps[:, 0:nsz]))
            dst, dtl = io["u_dst"](m)
            k.dma("sp", dst[:, n0:n0 + nsz], s[:, 0:nsz], [sl], dtl)

        linear(k, wpool, w[7:15], 8, 16, 128, rhs, CH, evac_u)
        k.barrier()


def phase_attn(k, c, io):
    scale = 192.0 ** -0.5
    with ExitStack() as es:
        cqn = k.sb(es, "cqn", [128, 4, T], BF16)
        cqn_tl = Tl("cqn")
        for kt in range(4):
            k.dma("sp", cqn[:, kt, :], io["cq_n"][kt * 128:(kt + 1) * 128, :], io["cq_n_tl"], [cqn_tl])
        kvn = k.sb(es, "kvn", [128, 2, LTOT], BF16)
        kvn_tl = Tl("kvn")
        for kt in range(2):
            for hh in range(2):
                src_, stl_ = io["kvx_half"](hh)
                k.dma("sp", kvn[:, kt, hh * T:(hh + 1) * T], src_[kt * 128:(kt + 1) * 128, :], stl_, [kvn_tl])
        kpe = k.sb(es, "kpe", [64, LTOT], BF16)
        kpe_tl = Tl("kpe")
        for hh in range(2):
            src_, stl_ = io["kvx_half"](hh)
            k.dma("sp", kpe[:, hh * T:(hh + 1) * T], src_[256:320, :], stl_, [kpe_tl])
        g_att, g_att_tl = load_small(k, es, "g_att", io["att_g"], [128, 8])
        ropeC, ropeC_tl = load_small(k, es, "ropeC", io["ropeC"], [64, T])
        ropeS, ropeS_tl = load_small(k, es, "ropeS", io["ropeS"], [64, T])
        wv = k.sb(es, "wv", [128, 2, 1024], BF16)
        wv_tl = Tl("wv")
        k.dma("pool", wv[:, 0, :], io["w_v"][:, 0, :], [], [wv_tl])
        k.dma("pool", wv[:, 1, :], io["w_v"][:, 1, :], [], [wv_tl])
        wpool = WPool(k, es, 4, kt=4)
        nt = NormTmp(k, es)
        HG = 2
        qn = k.sb(es, "qn", [128, HG, T], BF16)
        qn_tl = [Tl("qn") for _ in range(HG)]
        qr = k.sb(es, "qr", [64, HG, T], BF16)
        qr_tl = [Tl("qr") for _ in range(HG)]
        kn = k.sb(es, "kn", [128, HG, LTOT], BF16)
        kn_tl = [Tl("kn") for _ in range(HG)]
        vall = k.sb(es, "vall", [128, NKT, HG * 128], BF16)
        vall_tl = Tl("vall")
        att = k.sb(es, "att", [128, 8, T], F32)
        att_tl = [Tl("att") for _ in CH]
        tmpA = [(k.sb(es, "rtmp", [64, 344], F32), Tl("rtmp")) for _ in range(2)]
        tmpB = [(k.sb(es, "rtmp2", [64, 344], F32), Tl("rtmp2")) for _ in range(2)]
        pT = [(k.sb(es, "pT", [128, 344], BF16), Tl("pT")) for _ in range(3)]
        rden = [(k.sb(es, "rden", [128, 344], F32), Tl("rden")) for _ in range(2)]
        s_banks = [0, 1, 2]
        acc_banks = [(3, 4), (5, 6)]
        it = 0
        si_box = [0]
        for hg in range(8 // HG):
            for j in range(HG):
                hd = hg * HG + j
                slab, stl = wpool.get()
                k.dma("pool", slab[:, 0:4, :], io["w_uq_t"][2 * hd], [], [stl])
                for ci, (n0, nsz) in enumerate(CH):
                    ps, pst = k.psum[7], k.psum_tl[7]
                    for kt in range(4):
                        k.op("pe", [stl, cqn_tl], [pst],
                             lambda e: e.matmul(ps[:, 0:nsz], slab[:, kt, :], cqn[:, kt, n0:n0 + nsz],
                                                start=(kt == 0), stop=(kt == 3)))
                    k.op("act", [pst], [qn_tl[j]], lambda e: e.copy(qn[:, j, n0:n0 + nsz], ps[:, 0:nsz]))
                slab, stl = wpool.get()
                k.dma("pool", slab[:, 0:4, :], io["w_uq_t"][2 * hd + 1], [], [stl])
                for ci, (n0, nsz) in enumerate(CH):
                    psA, psAt = k.psum[7], k.psum_tl[7]
                    psB, psBt = k.psum[0], k.psum_tl[0]
                    for kt in range(4):
                        k.op("pe", [stl, cqn_tl], [psAt],
                             lambda e: e.matmul(psA[0:64, 0:nsz], slab[:, kt, 0:64], cqn[:, kt, n0:n0 + nsz],
                                                start=(kt == 0), stop=(kt == 3)))
                    for kt in range(4):
                        k.op("pe", [stl, cqn_tl], [psBt],
                             lambda e: e.matmul(psB[0:64, 0:nsz], slab[:, kt, 64:128], cqn[:, kt, n0:n0 + nsz],
                                                start=(kt == 0), stop=(kt == 3)))
                    rope_evac(k, psA, psAt, psB, psBt, ropeC, ropeC_tl, ropeS, ropeS_tl, tmpA[ci % 2], tmpB[ci % 2],
                              qr[:, j, n0:n0 + nsz], qr_tl[j], n0, nsz)
            for j in range(HG):
                hd = hg * HG + j
                slab, stl = wpool.get()
                k.dma("pool", slab[:, 0:2, :], io["w_uk_t"][hd], [], [stl])
                for ci, (n0, nsz) in enumerate(CHL):
                    bb = 7 if ci % 2 == 0 else 0
                    ps, pst = k.psum[bb], k.psum_tl[bb]
                    for kt in range(2):
                        k.op("pe", [stl, kvn_tl], [pst],
                             lambda e: e.matmul(ps[:, 0:nsz], slab[:, kt, :], kvn[:, kt, n0:n0 + nsz],
                                                start=(kt == 0), stop=(kt == 1)))
                    if ci % 2 == 0:
                        k.op("act", [pst], [kn_tl[j]], lambda e: e.copy(kn[:, j, n0:n0 + nsz], ps[:, 0:nsz]))
                    else:
                        k.op("dve", [pst], [kn_tl[j]], lambda e: e.tensor_copy(kn[:, j, n0:n0 + nsz], ps[:, 0:nsz]))
            VW = HG * 128
            for kt_ in range(NKT):
                k0 = kt_ * 128
                ksz = min(128, LTOT - k0)
                bb = 7 if kt_ % 2 == 0 else 0
                ps, pst = k.psum[bb], k.psum_tl[bb]
                for j2 in range(2):
                    k.op("pe", [kvn_tl, wv_tl], [pst],
                         lambda e: e.matmul(ps[0:ksz, 0:VW], kvn[:, j2, k0:k0 + ksz], wv[:, j2, hg * VW:(hg + 1) * VW],
                                            start=(j2 == 0), stop=(j2 == 1)))
                if kt_ % 2 == 0:
                    k.op("act", [pst], [vall_tl], lambda e: e.copy(vall[0:ksz, kt_, :], ps[0:ksz, 0:VW]))
                else:
                    k.op("dve", [pst], [vall_tl], lambda e: e.tensor_copy(vall[0:ksz, kt_, :], ps[0:ksz, 0:VW]))
            for j in range(HG):
                hd = hg * HG + j
                for ci, (n0, nsz) in enumerate(CH):
                    ob, db = acc_banks[it % 2]
                    po, pot = k.psum[ob], k.psum_tl[ob]
                    pd, pdt = k.psum[db], k.psum_tl[db]
                    def emit_s(kt_):
                        nonlocal_si = si_box[0]
                        k0 = kt_ * 128
                        ksz = min(128, LTOT - k0)
                        sb_ = s_banks[nonlocal_si % 3]
                        psc, psct = k.psum[sb_], k.psum_tl[sb_]
                        p, ptl = pT[nonlocal_si % 3]
                        si_box[0] += 1
                        k.op("pe", [kn_tl[j], qn_tl[j]], [psct],
                             lambda e: e.matmul(psc[0:ksz, 0:nsz], kn[:, j, k0:k0 + ksz], qn[:, j, n0:n0 + nsz],
                                                start=True, stop=False))
                        k.op("pe", [kpe_tl, qr_tl[j]], [psct],
                             lambda e: e.matmul(psc[0:ksz, 0:nsz], kpe[:, k0:k0 + ksz], qr[:, j, n0:n0 + nsz],
                                                start=False, stop=True))
                        k.op("act", [psct], [ptl],
                             lambda e: e.activation(p[0:ksz, 0:nsz], psc[0:ksz, 0:nsz], AF.Exp, scale=scale))
                        return (kt_, ksz, p, ptl)

                    def emit_pv(st):
                        kt_, ksz, p, ptl = st
                        k.op("pe", [ptl, vall_tl], [pot],
                             lambda e: e.matmul(po[:, 0:nsz], vall[0:ksz, kt_, j * 128:(j + 1) * 128], p[0:ksz, 0:nsz],
                                                start=(kt_ == 0), stop=(kt_ == NKT - 1)))
                        k.op("pe", [ptl, c.tl], [pdt],
                             lambda e: e.matmul(pd[:, 0:nsz], c.ones_b[0:ksz, :], p[0:ksz, 0:nsz],
                                                start=(kt_ == 0), stop=(kt_ == NKT - 1)))

                    pend = [emit_s(0)]
                    for kt_ in range(1, NKT):
                        pend.append(emit_s(kt_))
                        emit_pv(pend.pop(0))
                    emit_pv(pend.pop(0))
                    r, rtl = rden[it % 2]
                    k.op("dve", [pdt], [rtl], lambda e: e.reciprocal(r[:, 0:nsz], pd[:, 0:nsz]))
                    k.op("dve", [pot, rtl], [att_tl[ci]],
                         lambda e: e.tensor_tensor(att[:, hd, n0:n0 + nsz], po[:, 0:nsz], r[:, 0:nsz], ALU.mult))
                    it += 1
        k.ps_next = 0
        attn = k.sb(es, "attn", [128, 8, T], BF16)
        attn_tl = [Tl("attn") for _ in CH]
        rmsnorm(k, c, nt, att, att_tl, 8, CH, g_att, g_att_tl, attn, attn_tl, 1024)
        for kt in range(8):
            k.dma("sp", io["att_n"][kt * 128:(kt + 1) * 128, :], attn[:, kt, :], attn_tl, io["att_n_tl"])
        k.barrier()


def phase_s5(k, c, io):
    NP = NSL * 64
    with ExitStack() as es:
        rr = k.sb(es, "rr", [128, NGD], F32)
        phi = k.sb(es, "phi", [128, NGD], F32)
        sp_tl = Tl("s5par")
        lB = k.sb(es, "lB", [128, NSL, 128], BF16)
        lBs = k.sb(es, "lBs", [128, NSL, 128], BF16)
        lB_tl = Tl("lB")
        l1 = k.sb(es, "l1", [128, NGD, 32], BF16)
        l2 = k.sb(es, "l2", [128, NGD, 32], BF16)
        l12_tl = Tl("l12")
        with ExitStack() as ep:
            lre, lre_tl = load_small(k, ep, "lre", io["s5_lre_s"], [128, NGD])
            lim, lim_tl = load_small(k, ep, "lim", io["s5_lim_s"], [128, NGD])
            lst, lst_tl = load_small(k, ep, "lst", io["s5_ls_s"], [128, NGD])
            dt_s = k.sb(ep, "dt_s", [128, NGD], F32)
            dt_tl = Tl("dt_s")
            k.op("act", [lst_tl], [dt_tl], lambda e: e.activation(dt_s[:], lst[:], AF.Exp))
            k.op("dve", [lre_tl, dt_tl], [sp_tl], lambda e: e.tensor_tensor(rr[:], lre[:], dt_s[:], ALU.mult))
            k.op("act", [sp_tl], [sp_tl], lambda e: e.activation(rr[:], rr[:], AF.Exp))
            k.op("dve", [lim_tl, dt_tl, sp_tl], [sp_tl], lambda e: e.tensor_tensor(phi[:], lim[:], dt_s[:], ALU.mult))
            k.op("dve", [sp_tl], [sp_tl],
                 lambda e: e.tensor_scalar(phi[:], phi[:], 1.0 / (2.0 * math.pi), None, ALU.mult))
            lreb, lreb_tl = load_small(k, ep, "lreb", io["s5_lre_b"], [128, NP])
            limb, limb_tl = load_small(k, ep, "limb", io["s5_lim_b"], [128, NP])
            lsb, lsb_tl = load_small(k, ep, "lsb", io["s5_ls_b"], [128, NP])
            bre, bre_tl = load_small(k, ep, "bre", io["s5_bre"], [128, NP])
            bim, bim_tl = load_small(k, ep, "bim", io["s5_bim"], [128, NP])
            X = {}
            xt = Tl("s5tmp")
            for nm in ("dt", "mag", "tu", "fr", "sn", "cs", "ar", "ai", "den", "kr", "ki", "t1", "t2"):
                X[nm] = k.sb(ep, "x_" + nm, [128, NP], F32)
            ki32 = k.sb(ep, "ki32", [128, NP], I32)
            ins = [xt, lreb_tl, limb_tl, lsb_tl, bre_tl, bim_tl]

            def dv(fn):
                k.op("dve", ins, [xt], fn)

            def ac(fn):
                k.op("act", ins, [xt], fn)

            ac(lambda e: e.activation(X["dt"][:], lsb[:], AF.Exp))
            dv(lambda e: e.tensor_tensor(X["mag"][:], lreb[:], X["dt"][:], ALU.mult))
            ac(lambda e: e.activation(X["mag"][:], X["mag"][:], AF.Exp))
            dv(lambda e: e.tensor_tensor(X["tu"][:], limb[:], X["dt"][:], ALU.mult))
            dv(lambda e: e.tensor_scalar(X["tu"][:], X["tu"][:], 1.0 / (2.0 * math.pi), None, ALU.mult))

            def sin_turns(dst, src, off):
                if off != 0.0:
                    dv(lambda e: e.tensor_scalar(X["t1"][:], src[:], off, None, ALU.add))
                    s2 = X["t1"]
                else:
                    s2 = src
                dv(lambda e: e.tensor_copy(ki32[:], s2[:]))
                dv(lambda e: e.tensor_tensor(X["fr"][:], s2[:], ki32[:], ALU.subtract))
                ac(lambda e: e.activation(dst[:], X["fr"][:], AF.Sin, scale=TWO_PI_LO))

            sin_turns(X["sn"], X["tu"], 0.0)
            sin_turns(X["cs"], X["tu"], 0.25)
            dv(lambda e: e.tensor_tensor(X["ar"][:], X["mag"][:], X["cs"][:], ALU.mult))
            dv(lambda e: e.tensor_scalar(X["ar"][:], X["ar"][:], -1.0, None, ALU.add))
            dv(lambda e: e.tensor_tensor(X["ai"][:], X["mag"][:], X["sn"][:], ALU.mult))
            dv(lambda e: e.tensor_tensor(X["den"][:], lreb[:], lreb[:], ALU.mult))
            dv(lambda e: e.tensor_tensor(X["t1"][:], limb[:], limb[:], ALU.mult))
            dv(lambda e: e.tensor_tensor(X["den"][:], X["den"][:], X["t1"][:], ALU.add))
            dv(lambda e: e.reciprocal(X["den"][:], X["den"][:]))
            dv(lambda e: e.tensor_tensor(X["kr"][:], X["ar"][:], lreb[:], ALU.mult))
            dv(lambda e: e.tensor_tensor(X["t1"][:], X["ai"][:], limb[:], ALU.mult))
            dv(lambda e: e.tensor_tensor(X["kr"][:], X["kr"][:], X["t1"][:], ALU.add))
            dv(lambda e: e.tensor_tensor(X["kr"][:], X["kr"][:], X["den"][:], ALU.mult))
            dv(lambda e: e.tensor_tensor(X["ki"][:], X["ai"][:], lreb[:], ALU.mult))
            dv(lambda e: e.tensor_tensor(X["t1"][:], X["ar"][:], limb[:], ALU.mult))
            dv(lambda e: e.tensor_tensor(X["ki"][:], X["ki"][:], X["t1"][:], ALU.subtract))
            dv(lambda e: e.tensor_tensor(X["ki"][:], X["ki"][:], X["den"][:], ALU.mult))
            dv(lambda e: e.tensor_tensor(X["t1"][:], X["kr"][:], bre[:], ALU.mult))
            dv(lambda e: e.tensor_tensor(X["t2"][:], X["ki"][:], bim[:], ALU.mult))
            dv(lambda e: e.tensor_tensor(X["ar"][:], X["t1"][:], X["t2"][:], ALU.subtract))
            dv(lambda e: e.tensor_tensor(X["t1"][:], X["kr"][:], bim[:], ALU.mult))
            dv(lambda e: e.tensor_tensor(X["t2"][:], X["ki"][:], bre[:], ALU.mult))
            dv(lambda e: e.tensor_tensor(X["ai"][:], X["t1"][:], X["t2"][:], ALU.add))
            v3 = lambda a: a[:].rearrange("c (g p) -> c g p", p=64)
            k.op("dve", [xt], [lB_tl], lambda e: e.tensor_copy(lB[:, :, 0:64], v3(X["ar"])))
            k.op("dve", [xt], [lB_tl], lambda e: e.tensor_copy(lB[:, :, 64:128], v3(X["ai"])))
            k.op("dve", [xt], [lB_tl], lambda e: e.tensor_copy(lBs[:, :, 0:64], v3(X["ai"])))
            k.op("dve", [xt], [lB_tl], lambda e: e.tensor_scalar(lBs[:, :, 64:128], v3(X["ar"]), -1.0, None, ALU.mult))
            cta, cta_tl = load_small(k, ep, "cta", io["s5_cta"], [128, NGD, 16])
            ctb, ctb_tl = load_small(k, ep, "ctb", io["s5_ctb"], [128, NGD, 16])
            k.op("pool", [], [l12_tl], lambda e: e.memset(l1[:], 0.0))
            k.op("pool", [], [l12_tl], lambda e: e.memset(l2[:], 0.0))
            k.op("dve", [cta_tl, l12_tl], [l12_tl], lambda e: e.tensor_copy(l1[0:64, :, 0:16], cta[0:64]))
            k.op("dve", [cta_tl, l12_tl], [l12_tl],
                 lambda e: e.tensor_scalar(l1[64:128, :, 0:16], cta[64:128], -1.0, None, ALU.mult))
            k.op("dve", [ctb_tl, l12_tl], [l12_tl],
                 lambda e: e.tensor_scalar(l2[:, :, 0:16], ctb[:], -1.0, None, ALU.mult))
            k.barrier()
        Ubuf = [(k.sb(es, "U", [128, LTOT], BF16), Tl("U")) for _ in range(2)]
        Ua, Ua_tl = k.sb(es, "Ua", [128, LTOT], BF16), Tl("Ua")
        Ub, Ub_tl = k.sb(es, "Ub", [128, LTOT], BF16), Tl("Ub")
        msk, msk_tl = load_small(k, es, "msk", io["msk"], [128, 2])
        for ub_, ubl_ in Ubuf + [(Ua, Ua_tl), (Ub, Ub_tl)]:
            k.op("pool", [], [ubl_], lambda e: e.memset(ub_[:], 0.0))
        dsk, dsk_tl = load_small(k, es, "dsk", io["s5_d"], [128, NST])
        iot, iot_tl = load_small(k, es, "iot", io["iota_t"], [128, LTOT], dt=I16)
        gout = [(k.sb(es, "gout", [128, LTOT], F32), Tl("gout")) for _ in range(1)]
        gsel = [k.sb(es, "gsel", [128, T], F32) for _ in range(2)]
        gsel_tl = [Tl("gsel") for _ in range(2)]

        def mk(nm, dt, n):
            return [(k.sb(es, nm, [128, LTOT], dt), Tl(nm)) for _ in range(n)]
        tau = mk("tau", F32, 1) * 2
        gtmp, gtmp_tl = tau[0]
        kin = mk("kin", I16, 2)
        COS = mk("COS", BF16, 3)
        SIN = mk("SIN", BF16, 3)
        wbuf = mk("wbuf", BF16, 2)
        qbuf = mk("qbuf", BF16, 2)
        t1b = mk("t1b", BF16, 2)
        E1 = mk("E1", BF16, 2)
        E2 = mk("E2", BF16, 2)
        ybank = [4, 5, 6, 7, 3]

        def load_U(s):
            U, U_tl = Ubuf[s % 2]
            for q4 in range(3):
                gl = s * 3 + q4
                if gl < NG:
                    for hh in range(2):
                        k.dma("sp", Ua[32 * q4:32 * q4 + 16, hh * T:(hh + 1) * T],
                              io["u_loc"][gl * 16:(gl + 1) * 16, :], io["u_loc_tl"], [Ua_tl])
                        src_, stl_ = io["u_par"](hh)
                        k.dma("sp", Ub[32 * q4:32 * q4 + 16, hh * T:(hh + 1) * T], src_[gl * 16:(gl + 1) * 16, :], stl_, [Ub_tl])
            for hh in range(2):
                cs_ = slice(hh * T, (hh + 1) * T)
                k.op("dve", [Ua_tl, msk_tl], [Ua_tl],
                     lambda e: e.tensor_scalar(Ua[:, cs_], Ua[:, cs_], msk[:, hh:hh + 1], None, ALU.mult))
                k.op("dve", [Ua_tl, Ub_tl, msk_tl], [U_tl],
                     lambda e: e.scalar_tensor_tensor(U[:, cs_], Ub[:, cs_], msk[:, 1 - hh:2 - hh], Ua[:, cs_], ALU.mult, ALU.add))

        def stage_T(g, part):
            gd, i3 = g["gd"], g["idx"] % 3
            ta, tal = tau[0]

            def head(ti):
                ka, kal = kin[ti]
                k.op("act", [iot_tl, sp_tl], [tal],
                     lambda e: e.activation(ta[:], iot[:], AF.Identity, scale=phi[:, gd:gd + 1], bias=c.offs[:, ti:ti + 1]))
                k.op("act", [tal], [kal], lambda e: e.activation(ka[:], ta[:], AF.Identity))

            def tail(ti, dst):
                ka, kal = kin[ti]
                k.op("dve", [tal, kal], [tal], lambda e: e.tensor_tensor(ta[:], ta[:], ka[:], ALU.subtract))
                k.op("act", [tal], [dst[1]],
                     lambda e: e.activation(dst[0][:], ta[:], AF.Sin, scale=TWO_PI_LO))
            if part == 0:
                head(0)
                tail(0, SIN[i3])
                head(1)
            else:
                tail(1, COS[i3])

        def stage_I(g):
            s, q4, d, sd, i2 = g["s"], g["q4"], g["d"], g["sd"], g["idx"] % 2
            U, U_tl = Ubuf[s % 2]
            cs, cstl = COS[g["idx"] % 3]
            sn, sntl = SIN[g["idx"] % 3]
            w_, wtl = wbuf[i2]
            t1_, t1tl = t1b[i2]
            for ci, (n0, nsz) in enumerate(CHL):
                bA, bB = (2 * ci) % 3, (2 * ci + 1) % 3
                psA, psAt = k.psum[bA], k.psum_tl[bA]
                psB, psBt = k.psum[bB], k.psum_tl[bB]
                k.op("pe", [lB_tl, U_tl], [psAt],
                     lambda e: e.matmul(psA[:, 0:nsz], lB[32 * q4:32 * q4 + 16, sd, :],
                                        U[32 * q4:32 * q4 + 16, n0:n0 + nsz], start=True, stop=True))
                k.op("pe", [lB_tl, U_tl], [psBt],
                     lambda e: e.matmul(psB[:, 0:nsz], lBs[32 * q4:32 * q4 + 16, sd, :],
                                        U[32 * q4:32 * q4 + 16, n0:n0 + nsz], start=True, stop=True))
                if d == 0:
                    so = slice(n0, n0 + nsz)
                    inB = psB[:, 0:nsz]
                    outA = w_[:, so]
                else:
                    s0 = LTOT - n0 - nsz
                    so = slice(s0, s0 + nsz)
                    inB = psB[:, 0:nsz][:, ::-1]
                    outA = w_[:, so][:, ::-1]
                k.op("act", [psAt], [wtl], lambda e: e.copy(outA, psA[:, 0:nsz]))
                k.op("dve", [psBt, sntl], [t1tl],
                     lambda e: e.tensor_tensor(t1_[:, so], inB, sn[:, so], ALU.mult))
            k.op("dve", [wtl, cstl], [wtl], lambda e: e.tensor_tensor(w_[:], w_[:], cs[:], ALU.mult))
            k.op("dve", [t1tl, wtl], [wtl], lambda e: e.tensor_tensor(w_[:], w_[:], t1_[:], ALU.add))

        def stage_S(g):
            gd, i2 = g["gd"], g["idx"] % 2
            w_, wtl = wbuf[i2]
            qb, qtl = qbuf[i2]
            k.op("dve", [wtl, sp_tl], [qtl],
                 lambda e: e.tensor_tensor_scan(qb[:], rr[:, gd:gd + 1].broadcast_to([128, LTOT]), w_[:], 0.0,
                                                ALU.mult, ALU.add))

        def stage_O(g):
            q4, d, gd, i2 = g["q4"], g["d"], g["gd"], g["idx"] % 2
            cs, cstl = COS[g["idx"] % 3]
            sn, sntl = SIN[g["idx"] % 3]
            qb, qtl = qbuf[i2]
            e1, e1tl = E1[i2]
            e2, e2tl = E2[i2]
            o1 = e1[:, ::-1] if d == 1 else e1[:]
            o2 = e2[:, ::-1] if d == 1 else e2[:]
            k.op("dve", [qtl, cstl], [e1tl], lambda e: e.tensor_tensor(o1, qb[:], cs[:], ALU.mult))
            k.op("dve", [qtl, sntl], [e2tl], lambda e: e.tensor_tensor(o2, qb[:], sn[:], ALU.mult))
            for ci, (n0, nsz) in enumerate(CHL):
                py, pyt = k.psum[ybank[ci]], k.psum_tl[ybank[ci]]
                oap = py[32 * q4:32 * q4 + 32, 0:nsz]
                k.op("pe", [l12_tl, e1tl], [pyt],
                     lambda e: e.matmul(oap, l1[:, gd, :], e1[:, n0:n0 + nsz], start=(d == 0), stop=False))
                k.op("pe", [l12_tl, e2tl], [pyt],
                     lambda e: e.matmul(oap, l2[:, gd, :], e2[:, n0:n0 + nsz], start=False, stop=(d == 1)))

        def evac_tile(s):
            U, U_tl = Ubuf[s % 2]
            go, gotl = gout[0]
            PV = 96 if s < NST - 1 else 64
            for ci, (n0, nsz) in enumerate(CHL):
                py, pyt = k.psum[ybank[ci]], k.psum_tl[ybank[ci]]
                k.op("dve", [pyt, U_tl, dsk_tl], [gotl],
                     lambda e: e.scalar_tensor_tensor(go[0:PV, n0:n0 + nsz], U[0:PV, n0:n0 + nsz], dsk[0:PV, s:s + 1],
                                                      py[0:PV, 0:nsz], ALU.mult, ALU.add))
            k.op("act", [gotl], [gtmp_tl], lambda e: e.activation(gtmp[0:PV], go[0:PV], AF.Square))
            k.op("dve", [gtmp_tl], [gtmp_tl],
                 lambda e: e.tensor_scalar(gtmp[0:PV], gtmp[0:PV], 0.044715, 1.0, ALU.mult, ALU.add))
            k.op("dve", [gtmp_tl, gotl], [gtmp_tl], lambda e: e.tensor_tensor(gtmp[0:PV], gtmp[0:PV], go[0:PV], ALU.mult))
            k.op("act", [gtmp_tl], [gtmp_tl], lambda e: e.activation(gtmp[0:PV], gtmp[0:PV], AF.Sigmoid, scale=1.5957691216))
            k.op("dve", [gtmp_tl, gotl], [gotl], lambda e: e.tensor_tensor(go[0:PV], go[0:PV], gtmp[0:PV], ALU.mult))
            g0, g1 = go[0:PV, 0:T], go[0:PV, T:2 * T]
            k.op("dve", [gotl, msk_tl], [gsel_tl[0]],
                 lambda e: e.tensor_scalar(gsel[0][0:PV, :], g0, msk[0:PV, 0:1], None, ALU.mult))
            k.op("dve", [gotl, msk_tl, gsel_tl[0]], [gsel_tl[0]],
                 lambda e: e.scalar_tensor_tensor(gsel[0][0:PV, :], g1, msk[0:PV, 1:2], gsel[0][0:PV, :], ALU.mult, ALU.add))
            k.op("dve", [gotl, msk_tl], [gsel_tl[1]],
                 lambda e: e.tensor_scalar(gsel[1][0:PV, :], g0, msk[0:PV, 1:2], None, ALU.mult))
            k.op("dve", [gotl, msk_tl, gsel_tl[1]], [gsel_tl[1]],
                 lambda e: e.scalar_tensor_tensor(gsel[1][0:PV, :], g1, msk[0:PV, 0:1], gsel[1][0:PV, :], ALU.mult, ALU.add))
            for q4 in range(3):
                gl = s * 3 + q4
                if gl < NG:
                    for which in range(2):
                        dst, dtl = io["g_dst"](which, gl)
                        k.dma("sp", dst, gsel[which][32 * q4:32 * q4 + 16, :], [gsel_tl[which]], dtl)

        gds = []
        for s in range(NST):
            tile_g = [(q4, d) for q4 in range(3) if s * 3 + q4 < NG for d in range(2)]
            for j, (q4, d) in enumerate(tile_g):
                gl = s * 3 + q4
                gds.append({"s": s, "q4": q4, "d": d, "gd": d * NG + gl, "sd": s * 2 + d, "idx": len(gds),
                            "first": j == 0, "last": j == len(tile_g) - 1})
        load_U(0)
        stage_T(gds[0], 0)
        stage_T(gds[0], 1)
        for i, g in enumerate(gds):
            stage_I(g)
            if i + 1 < len(gds):
                stage_T(gds[i + 1], 0)
            if i >= 1:
                pg = gds[i - 1]
                stage_S(pg)
                stage_O(pg)
            if i + 1 < len(gds):
                stage_T(gds[i + 1], 1)
            if i >= 1 and gds[i - 1]["last"]:
                evac_tile(gds[i - 1]["s"])
            if g["first"] and g["s"] + 1 < NST:
                load_U(g["s"] + 1)
        pg = gds[-1]
        stage_S(pg)
        stage_O(pg)
        evac_tile(pg["s"])
        k.ps_next = 0
        k.barrier()


def moe_gates(k, c, es, io, h, h_tl, g_ffn, g_ffn_tl, rsk, rsk_tl):
    wr, wr_tl = load_small(k, es, "wr", io["router"], [128, 16, 8])
    ident, ident_tl = load_small(k, es, "ident", io["ident"], [128, 128])
    sel, sel_tl = load_small(k, es, "sel", io["sel"], [8, 8, 128])
    k.op("dve", [wr_tl, g_ffn_tl], [wr_tl],
         lambda e: e.tensor_tensor(wr[:], wr[:], g_ffn[:].unsqueeze(2).broadcast_to([128, 16, 8]), ALU.mult))
    GT = k.sb(es, "GT", [8, T], F32)
    GT_tl = Tl("GT")
    W = {}
    wl = Tl("gw")
    for nm, wd in (("lg", 16), ("lgs", 8), ("m8", 8), ("mk", 8), ("ntp", 1), ("ex", 8), ("em", 8), ("dn", 1), ("gt", 8)):
        W[nm] = k.sb(es, "gw_" + nm, [128, wd], F32)
    ntile = (T + 127) // 128
    for tt in range(ntile):
        t0 = tt * 128
        tsz = min(128, T - t0)
        ci = next(i for i, (n0, nsz) in enumerate(CH) if n0 <= t0 < n0 + nsz)
        ci2 = next(i for i, (n0, nsz) in enumerate(CH) if n0 <= t0 + tsz - 1 < n0 + nsz)
        deps_h = [h_tl[ci]] + ([h_tl[ci2]] if ci2 != ci else [])
        deps_r = [rsk_tl[ci]] + ([rsk_tl[ci2]] if ci2 != ci else [])
        ps, pst = k.ps()
        for kt in range(16):
            k.op("pe", deps_h + [wr_tl], [pst],
                 lambda e: e.matmul(ps[0:tsz, 0:8], h[:, kt, t0:t0 + tsz], wr[:, kt, :],
                                    start=(kt == 0), stop=(kt == 15)))
        ps2, ps2t = k.ps()
        k.op("pe", deps_r + [c.tl], [ps2t],
             lambda e: e.matmul(ps2[0:tsz, 0:2], rsk[0:1, t0:t0 + tsz], c.ones_f[0:1, 0:2], start=True, stop=True))
        k.op("dve", [pst, wl], [wl], lambda e: e.tensor_copy(W["lg"][0:tsz, 0:8], ps[0:tsz, 0:8]))
        k.op("dve", [ps2t, wl], [wl], lambda e: e.tensor_copy(W["lg"][0:tsz, 8:10], ps2[0:tsz, 0:2]))
        k.op("dve", [wl], [wl],
             lambda e: e.tensor_scalar(W["lgs"][0:tsz, :], W["lg"][0:tsz, 0:8], W["lg"][0:tsz, 8:9], None, ALU.mult))
        k.op("dve", [wl], [wl], lambda e: e.max(W["m8"][0:tsz, :], W["lgs"][0:tsz, :]))
        k.op("dve", [wl], [wl],
             lambda e: e.tensor_scalar(W["mk"][0:tsz, :], W["lgs"][0:tsz, :], W["m8"][0:tsz, 1:2], None, ALU.is_ge))
        k.op("dve", [wl], [wl],
             lambda e: e.tensor_scalar(W["ntp"][0:tsz, :], W["m8"][0:tsz, 0:1], -1.0, None, ALU.mult))
        k.op("act", [wl], [wl],
             lambda e: e.activation(W["ex"][0:tsz, :], W["lgs"][0:tsz, :], AF.Exp, bias=W["ntp"][0:tsz, 0:1]))
        k.op("dve", [wl], [wl], lambda e: e.tensor_tensor(W["em"][0:tsz, :], W["ex"][0:tsz, :], W["mk"][0:tsz, :], ALU.mult))
        k.op("dve", [wl], [wl], lambda e: e.reduce_sum(W["dn"][0:tsz, :], W["em"][0:tsz, :], AX.X))
        k.op("dve", [wl], [wl], lambda e: e.reciprocal(W["dn"][0:tsz, :], W["dn"][0:tsz, :]))
        k.op("dve", [wl], [wl],
             lambda e: e.tensor_scalar(W["gt"][0:tsz, :], W["em"][0:tsz, :], W["dn"][0:tsz, 0:1], None, ALU.mult))
        ps3, ps3t = k.ps()
        k.op("pe", [wl, ident_tl], [ps3t],
             lambda e: e.transpose(ps3[0:8, 0:tsz], W["gt"][0:tsz, 0:8], ident[0:tsz, 0:tsz]))
        k.op("act", [ps3t], [GT_tl], lambda e: e.copy(GT[0:8, t0:t0 + tsz], ps3[0:8, 0:tsz]))
    return GT, GT_tl, sel, sel_tl


def phase_c(k, c, io, h, h_tl, kind, final):
    NE = 4 if kind == "dense" else 8
    with ExitStack() as es:
        wpool = WPool(k, es, 5)
        nt = NormTmp(k, es)
        g_ffn, g_ffn_tl = load_small(k, es, "g_ffn", io["ffn_g"], [128, 16])
        with ExitStack() as e1:
            g_ssm, g_ssm_tl = load_small(k, e1, "g_ssm", io["ssm_g"], [128, 8])
            mixed = k.sb(e1, "mixed", [128, 16, T], BF16)
            mixed_tl = [Tl("mixed") for _ in CH]
            for kt in range(8):
                k.dma("sp", mixed[:, kt, :], io["att_n"][kt * 128:(kt + 1) * 128, :], io["att_n_tl"], mixed_tl)
            gf = k.sb(e1, "gf", [128, 8, T], F32)
            gf_tl = [Tl("gf") for _ in CH]
            msk, msk_tl = load_small(k, e1, "msk", io["msk"], [128, 2])
            for kt in range(4):
                k.dma("sp", gf[:, kt, :], io["g_loc"][kt * 128:(kt + 1) * 128, :], io["g_loc_tl"], gf_tl)
            gpa = [(k.sb(e1, "gpa", [128, T], F32), Tl("gpa")) for _ in range(2)]
            gpb = [(k.sb(e1, "gpb", [128, T], F32), Tl("gpb")) for _ in range(2)]
            for kt in range(4):
                a_, atl = gpa[kt % 2]
                b_, btl = gpb[kt % 2]
                s0, s0tl = io["g_par"](0, kt)
                s1, s1tl = io["g_par"](1, kt)
                k.dma("sp", a_[:], s0, s0tl, [atl])
                k.dma("sp", b_[:], s1, s1tl, [btl])
                k.op("dve", [atl, msk_tl], [atl], lambda e: e.tensor_scalar(a_[:], a_[:], msk[:, 1:2], None, ALU.mult))
                k.op("dve", [atl, btl, msk_tl], gf_tl,
                     lambda e: e.scalar_tensor_tensor(gf[:, 4 + kt, :], b_[:], msk[:, 0:1], a_[:], ALU.mult, ALU.add))
            gb = k.sb(e1, "gb", [128, 8, T], BF16)
            gb_tl = [Tl("gb") for _ in CH]
            for ci, (n0, nsz) in enumerate(CH):
                for kt in range(8):
                    if kt % 2 == 0:
                        k.op("act", [gf_tl[ci]], [gb_tl[ci]], lambda e: e.copy(gb[:, kt, n0:n0 + nsz], gf[:, kt, n0:n0 + nsz]))
                    else:
                        k.op("dve", [gf_tl[ci]], [gb_tl[ci]], lambda e: e.tensor_copy(gb[:, kt, n0:n0 + nsz], gf[:, kt, n0:n0 + nsz]))
            sig = [(k.sb(e1, "sig", [128, 344], F32), Tl("sig")) for _ in range(2)]
            cnt = [0]

            def evac_glu(m, ci, ps, pst, n0, nsz):
                sg, sgl = sig[cnt[0] % 2]
                cnt[0] += 1
                k.op("act", [pst], [sgl], lambda e: e.activation(sg[:, 0:nsz], ps[:, 0:nsz], AF.Sigmoid))
                k.op("dve", [sgl, gf_tl[ci]], [gf_tl[ci]],
                     lambda e: e.tensor_tensor(gf[:, m, n0:n0 + nsz], gf[:, m, n0:n0 + nsz], sg[:, 0:nsz], ALU.mult))

            linear(k, wpool, io["w_glu_t"], 8, 8, 128,
                   lambda kt, ci: (gb[:, kt, CH[ci][0]:CH[ci][0] + CH[ci][1]], gb_tl[ci]), CH, evac_glu)
            rmsnorm(k, c, nt, gf, gf_tl, 8, CH, g_ssm, g_ssm_tl, mixed, mixed_tl, 1024, out_off=8)

            def evac_out(m, ci, ps, pst, n0, nsz):
                k.op("dve", [pst, h_tl[ci]], [h_tl[ci]],
                     lambda e: e.tensor_tensor(h[:, m, n0:n0 + nsz], h[:, m, n0:n0 + nsz], ps[:, 0:nsz], ALU.add))

            linear(k, wpool, io["w_out_t"], 16, 16, 128,
                   lambda kt, ci: (mixed[:, kt, CH[ci][0]:CH[ci][0] + CH[ci][1]], mixed_tl[ci]), CH, evac_out)
            k.barrier()
        with ExitStack() as e2:
            hn = k.sb(e2, "hn2", [128, 16, T], BF16)
            hn_tl = [Tl("hn2") for _ in CH]
            rsk = k.sb(e2, "rsk", [128, T], F32)
            rsk_tl = [Tl("rsk") for _ in CH]
            rmsnorm(k, c, nt, h, h_tl, 16, CH, g_ffn, g_ffn_tl, hn, hn_tl, D, keep=(rsk, rsk_tl))
            gbt = None
            if kind == "moe":
                gbt = moe_gates(k, c, e2, io, h, h_tl, g_ffn, g_ffn_tl, rsk, rsk_tl)
            hid = k.sb(e2, "hid", [128, 11, T], BF16)
            hid_tl = [Tl("hid") for _ in CH]
            sil = [(k.sb(e2, "sil", [128, 344], F32), Tl("sil")) for _ in range(2)]
            tt = [(k.sb(e2, "tt", [128, 344], F32), Tl("tt")) for _ in range(2)]
            gbc = [(k.sb(e2, "gbc", [128, T], F32), Tl("gbc")) for _ in range(2)] if kind == "moe" else None
            n_ev = [0]
            for ex in range(NE):
                gcur = None
                if kind == "moe":
                    gcur = gbc[ex % 2]
                    GT, GT_tl, sel, sel_tl = gbt
                    for ci, (n0, nsz) in enumerate(CH):
                        ps, pst = k.ps()
                        k.op("pe", [GT_tl, sel_tl], [pst],
                             lambda e: e.matmul(ps[:, 0:nsz], sel[0:8, ex, :], GT[0:8, n0:n0 + nsz], start=True, stop=True))
                        k.op("act", [pst], [gcur[1]], lambda e: e.copy(gcur[0][:, n0:n0 + nsz], ps[:, 0:nsz]))
                for m in range(11):
                    sg_, sgtl = wpool.get()
                    su_, sutl = wpool.get()
                    k.dma("pool", sg_[:, 0:16, :], io["wg_t"][ex, m], [], [sgtl])
                    k.dma("pool", su_[:, 0:16, :], io["wu_t"][ex, m], [], [sutl])
                    for ci, (n0, nsz) in enumerate(CH):
                        pg, pgt = k.ps()
                        pu, put = k.ps()
                        for kt in range(16):
                            k.op("pe", [sgtl, hn_tl[ci]], [pgt],
                                 lambda e: e.matmul(pg[:, 0:nsz], sg_[:, kt, :], hn[:, kt, n0:n0 + nsz],
                                                    start=(kt == 0), stop=(kt == 15)))
                        for kt in range(16):
                            k.op("pe", [sutl, hn_tl[ci]], [put],
                                 lambda e: e.matmul(pu[:, 0:nsz], su_[:, kt, :], hn[:, kt, n0:n0 + nsz],
                                                    start=(kt == 0), stop=(kt == 15)))
                        sl, sltl = sil[n_ev[0] % 2]
                        k.op("act", [pgt], [sltl], lambda e: e.activation(sl[:, 0:nsz], pg[:, 0:nsz], AF.Silu))
                        if kind == "dense":
                            k.op("dve", [put, sltl], [hid_tl[ci]],
                                 lambda e: e.tensor_tensor(hid[:, m, n0:n0 + nsz], pu[:, 0:nsz], sl[:, 0:nsz], ALU.mult))
                        else:
                            t_, ttl = tt[n_ev[0] % 2]
                            k.op("dve", [put, sltl], [ttl],
                                 lambda e: e.tensor_tensor(t_[:, 0:nsz], pu[:, 0:nsz], sl[:, 0:nsz], ALU.mult))
                            k.op("dve", [ttl, gcur[1]], [hid_tl[ci]],
                                 lambda e: e.tensor_tensor(hid[:, m, n0:n0 + nsz], t_[:, 0:nsz], gcur[0][:, n0:n0 + nsz], ALU.mult))
                        n_ev[0] += 1

                def evac_down(m, ci, ps, pst, n0, nsz):
                    k.op("dve", [pst, h_tl[ci]], [h_tl[ci]],
                         lambda e: e.tensor_tensor(h[:, m, n0:n0 + nsz], h[:, m, n0:n0 + nsz], ps[:, 0:nsz], ALU.add))

                linear(k, wpool, io["wd_t"][ex], 16, 11, 128,
                       lambda kt, ci: (hid[:, kt, CH[ci][0]:CH[ci][0] + CH[ci][1]], hid_tl[ci]), CH, evac_down)
            k.barrier()
        if final:
            with ExitStack() as e3:
                g_fin, g_fin_tl = load_small(k, e3, "g_fin", io["fin_g"], [128, 16])
                stg = [(k.sb(e3, "ostg", [128, 16, 344], F32), Tl("ostg")) for _ in range(1)]
                o, otl = stg[0]
                for ci, (n0, nsz) in enumerate(CH):
                    rmsnorm(k, c, nt, h[:, :, n0:n0 + nsz], [h_tl[ci]], 16, [(0, nsz)], g_fin, g_fin_tl, o, [otl], D)
                    for kt in range(16):
                        k.dma("sp", io["hT_out"][kt * 128:(kt + 1) * 128, n0:n0 + nsz], o[:, kt, 0:nsz], [otl], [])
                k.barrier()


RG_PAIRS = [[0, 1], [2, 3], [4, 5], [6, 7]]
X1R = 832


def fused_inputs():
    f, L = "f", DEPTH
    ins = {"hT": ([D, T], f), "ropeC": ([64, T], f), "ropeS": ([64, T], f), "iota_t": ([128, LTOT], "i"),
           "ident": ([128, 128], f), "sel": ([8, 8, 128], f), "fin_g": ([128, 16], f),
           "w_in_t": ([L, 15, 128, 16, 128], f), "mix_g": ([L, 128, 16], f), "q_g": ([L, 128, 4], f),
           "kv_g": ([L, 128, 2], f), "w_uq_t": ([L, 16, 128, 4, 128], f), "w_uk_t": ([L, 8, 128, 2, 128], f),
           "w_v": ([L, 128, 2, 1024], f), "att_g": ([L, 128, 8], f),
           "s5_lre_s": ([L, 128, NGD], f), "s5_lim_s": ([L, 128, NGD], f), "s5_ls_s": ([L, 128, NGD], f),
           "s5_lre_b": ([L, 128, NSL * 64], f), "s5_lim_b": ([L, 128, NSL * 64], f), "s5_ls_b": ([L, 128, NSL * 64], f),
           "s5_bre": ([L, 128, NSL * 64], f), "s5_bim": ([L, 128, NSL * 64], f),
           "s5_cta": ([L, 128, NGD, 16], f), "s5_ctb": ([L, 128, NGD, 16], f), "s5_d": ([L, 128, NST], f),
           "ssm_g": ([L, 128, 8], f), "ffn_g": ([L, 128, 16], f),
           "w_glu_t": ([L, 8, 128, 8, 128], f), "w_out_t": ([L, 16, 128, 16, 128], f),
           "wg_d": ([2, 4, 11, 128, 16, 128], f), "wu_d": ([2, 4, 11, 128, 16, 128], f), "wd_d": ([2, 4, 16, 128, 11, 128], f),
           "wg_m": ([2, 8, 11, 128, 16, 128], f), "wu_m": ([2, 8, 11, 128, 16, 128], f), "wd_m": ([2, 8, 16, 128, 11, 128], f),
           "router": ([2, 128, 16, 8], f), "msk": ([128, 2], f)}
    return ins


class FusedCtx:
    pass


def load_h(k, es, io):
    h = k.sb(es, "h", [128, 16, T], F32)
    h_tl = [Tl("h") for _ in CH]
    for kt in range(16):
        k.dma("sp", h[:, kt, :], io["hT"][kt * 128:(kt + 1) * 128, :], [], h_tl)
    return h, h_tl


def _dt(name):
    return {"f": F32, "b": BF16, "i": I16}[name]


def build_fused(nlayers=DEPTH):
    nc = bass.Bass("TRN2", target_bir_lowering=False)
    ins = fused_inputs()
    io = {}
    for nm, (shape, dt) in ins.items():
        io[nm] = nc.dram_tensor(nm, shape, _dt(dt), kind="ExternalInput").ap()
    io["hT_out"] = nc.dram_tensor("hT_out", [D, T], F32, kind="ExternalOutput").ap()
    x1_in = nc.dram_tensor("x1_in", [X1R, T], BF16)
    x1_out = nc.dram_tensor("x1_out", [2 * X1R, T], BF16)
    x2_in = [nc.dram_tensor("x2_in%d" % i, [256, T], F32) for i in range(2)]
    x2_out = [nc.dram_tensor("x2_out%d" % i, [512, T], F32) for i in range(2)]
    cq_s = nc.dram_tensor("cq_s", [512, T], BF16)
    att_s = nc.dram_tensor("att_s", [1024, T], BF16)
    u_loc = nc.dram_tensor("u_loc", [512, T], BF16)
    g_loc = nc.dram_tensor("g_loc", [512, T], F32)
    tl = {n: Tl(n) for n in ("x1_in", "x1_out", "x2_in0", "x2_in1", "x2_out0", "x2_out1", "cq", "att", "uloc", "gloc")}
    with ExitStack() as es:
        k = K(nc, es)
        c = make_consts(k, es)
        h, h_tl = load_h(k, es, io)
        for l in range(nlayers):
            kind = "dense" if l % 2 == 0 else "moe"
            final = (l == DEPTH - 1)
            ioa = {"mix_g": io["mix_g"][l], "q_g": io["q_g"][l], "kv_g": io["kv_g"][l], "ropeC": io["ropeC"],
                   "ropeS": io["ropeS"], "w_in_t": io["w_in_t"][l], "cq_n": cq_s.ap(), "cq_n_tl": [tl["cq"]],
                   "kvx": x1_in.ap(), "kvx_tl": [tl["x1_in"]]}

            def u_dst(m):
                if m < 4:
                    return u_loc.ap()[m * 128:(m + 1) * 128, :], [tl["uloc"]]
                return x1_in.ap()[320 + (m - 4) * 128:320 + (m - 3) * 128, :], [tl["x1_in"]]
            ioa["u_dst"] = u_dst
            phase_a(k, c, ioa, h, h_tl)
            k.collective(x1_in, x1_out, [tl["x1_in"]], [tl["x1_out"]], RG_PAIRS)
            iob = {"cq_n": cq_s.ap(), "cq_n_tl": [tl["cq"]], "att_g": io["att_g"][l], "ropeC": io["ropeC"],
                   "ropeS": io["ropeS"], "w_v": io["w_v"][l], "w_uq_t": io["w_uq_t"][l], "w_uk_t": io["w_uk_t"][l],
                   "att_n": att_s.ap(), "att_n_tl": [tl["att"]], "iota_t": io["iota_t"], "msk": io["msk"],
                   "u_loc": u_loc.ap(), "u_loc_tl": [tl["uloc"]]}
            for nm in ("s5_lre_s", "s5_lim_s", "s5_ls_s", "s5_lre_b", "s5_lim_b", "s5_ls_b", "s5_bre", "s5_bim",
                       "s5_cta", "s5_ctb", "s5_d"):
                iob[nm] = io[nm][l]
            iob["kvx_half"] = lambda hh: (x1_out.ap()[hh * X1R:hh * X1R + 320, :], [tl["x1_out"]])
            iob["u_par"] = lambda hh: (x1_out.ap()[hh * X1R + 320:(hh + 1) * X1R, :], [tl["x1_out"]])

            def g_dst(which, gl):
                if which == 0:
                    return g_loc.ap()[gl * 16:(gl + 1) * 16, :], [tl["gloc"]]
                i2 = gl // 16
                r0 = (gl % 16) * 16
                return x2_in[i2].ap()[r0:r0 + 16, :], [tl["x2_in%d" % i2]]
            iob["g_dst"] = g_dst
            phase_attn(k, c, iob)
            phase_s5(k, c, iob)
            for i2 in range(2):
                k.collective(x2_in[i2], x2_out[i2], [tl["x2_in%d" % i2]], [tl["x2_out%d" % i2]], RG_PAIRS)
            ioc = {"att_n": att_s.ap(), "att_n_tl": [tl["att"]], "ssm_g": io["ssm_g"][l], "ffn_g": io["ffn_g"][l],
                   "w_glu_t": io["w_glu_t"][l], "w_out_t": io["w_out_t"][l], "msk": io["msk"],
                   "g_loc": g_loc.ap(), "g_loc_tl": [tl["gloc"]], "hT_out": io["hT_out"], "fin_g": io["fin_g"],
                   "ident": io["ident"], "sel": io["sel"]}
            sfx = "d" if kind == "dense" else "m"
            ioc["wg_t"], ioc["wu_t"], ioc["wd_t"] = io["wg_" + sfx][l // 2], io["wu_" + sfx][l // 2], io["wd_" + sfx][l // 2]
            if kind == "moe":
                ioc["router"] = io["router"][l // 2]
            ioc["g_par"] = lambda blk, kt: (x2_out[kt // 2].ap()[blk * 256 + (kt % 2) * 128:blk * 256 + (kt % 2) * 128 + 128, :],
                                            [tl["x2_out%d" % (kt // 2)]])
            phase_c(k, c, ioc, h, h_tl, kind, final or (l == nlayers - 1 and nlayers < DEPTH))
        k.finish()
    return nc


def tile_w(W, mw=128):
    Kd, Md = W.shape
    KT, MT = Kd // 128, Md // mw
    return np.ascontiguousarray(W.reshape(KT, 128, MT, mw).transpose(2, 1, 0, 3))


def col_gain(g):
    n = g.shape[0] // 128
    return np.ascontiguousarray(g.reshape(n, 128).T)


def rope_consts():
    inv = (10000.0 ** (-np.arange(0, 64, 2, dtype=np.float32) / 64)).astype(np.float32)
    ang = np.arange(LTOT, dtype=np.float32)[:, None] * inv[None, :]
    cos, sin = np.cos(ang).astype(np.float32).T, np.sin(ang).astype(np.float32).T
    C = np.concatenate([cos, cos], 0)
    S = np.concatenate([-sin, sin], 0)
    return np.ascontiguousarray(C), np.ascontiguousarray(S)


def swap_halves(Wc):
    return np.concatenate([Wc[:, 32:64], Wc[:, 0:32]], axis=1)


def chan_perm(hf):
    own = np.arange(hf * 512, (hf + 1) * 512)
    par = np.arange((1 - hf) * 512, (2 - hf) * 512)
    return np.concatenate([own, par])


def prep_shared(inp):
    P = {}
    L = DEPTH
    uq, uk, wv = [], [], []
    for l in range(L):
        wq = inp["w_uq"][l]
        cols = []
        for hd in range(8):
            b = hd * 192
            r = wq[:, b + 128:b + 192]
            cols += [wq[:, b:b + 128], r, swap_halves(r)]
        uq.append(tile_w(np.concatenate(cols, axis=1)))
        wkv = inp["w_ukv"][l]
        uk.append(tile_w(np.concatenate([wkv[:, hd * 256:hd * 256 + 128] for hd in range(8)], axis=1)))
        v = np.concatenate([wkv[:, hd * 256 + 128:hd * 256 + 256] for hd in range(8)], axis=1)
        wv.append(np.ascontiguousarray(v.reshape(2, 128, 1024).transpose(1, 0, 2)))
    P["w_uq_t"], P["w_uk_t"], P["w_v"] = np.stack(uq), np.stack(uk), np.stack(wv)
    for nm, key in (("mix_g", "mix_norm"), ("q_g", "q_norm"), ("kv_g", "kv_norm"), ("att_g", "attn_out_norm"),
                    ("ffn_g", "ffn_norm")):
        P[nm] = np.stack([col_gain(inp[key][l]) for l in range(L)])
    P["fin_g"] = col_gain(inp["final_norm"])
    for sfx, g, u, d, ne in (("d", "dense_w_gate", "dense_w_up", "dense_w_down", 4), ("m", "moe_w_gate", "moe_w_up", "moe_w_down", 8)):
        wg, wu, wd = [], [], []
        for i in range(2):
            if sfx == "d":
                G, U, Dn = inp[g][i], inp[u][i], inp[d][i]
                wg.append(np.stack([tile_w(G[:, e * 1408:(e + 1) * 1408]) for e in range(4)]))
                wu.append(np.stack([tile_w(U[:, e * 1408:(e + 1) * 1408]) for e in range(4)]))
                wd.append(np.stack([tile_w(Dn[e * 1408:(e + 1) * 1408, :]) for e in range(4)]))
            else:
                wg.append(np.stack([tile_w(inp[g][i][e]) for e in range(8)]))
                wu.append(np.stack([tile_w(inp[u][i][e]) for e in range(8)]))
                wd.append(np.stack([tile_w(inp[d][i][e]) for e in range(8)]))
        P["wg_" + sfx], P["wu_" + sfx], P["wd_" + sfx] = np.stack(wg), np.stack(wu), np.stack(wd)
    P["router"] = np.stack([np.ascontiguousarray(inp["moe_router"][i].reshape(16, 128, 8).transpose(1, 0, 2)) for i in range(2)])
    return P


def prep_half(inp, hf):
    P = {}
    perm = chan_perm(hf)
    win, wglu, wout, ssmg = [], [], [], []
    for l in range(DEPTH):
        w_in = inp["w_in"][l]
        kr = w_in[:, 768:832]
        w_in_x = np.concatenate([w_in[:, 0:768], kr, swap_halves(kr), w_in[:, 832:][:, perm]], axis=1)
        win.append(tile_w(w_in_x))
        wglu.append(tile_w(inp["ssm_w_glu"][l][perm][:, perm]))
        wo = inp["w_out"][l]
        wout.append(tile_w(np.concatenate([wo[0:1024], wo[1024:][perm]], axis=0)))
        ssmg.append(col_gain(inp["ssm_out_norm"][l][perm]))
    P["w_in_t"], P["w_glu_t"], P["w_out_t"], P["ssm_g"] = np.stack(win), np.stack(wglu), np.stack(wout), np.stack(ssmg)
    s5 = [prep_s5(inp, l, hf) for l in range(DEPTH)]
    for nm in s5[0]:
        P[nm] = np.stack([s5[l][nm] for l in range(DEPTH)])
    m = np.zeros((128, 2), np.float32)
    m[:, hf] = 1.0
    P["msk"] = m
    return P


def prep_s5(inp, l, hf):
    G0 = hf * NG
    lre = inp["ssm_lambda_re"][l][:, G0:G0 + NG]
    lim = inp["ssm_lambda_im"][l][:, G0:G0 + NG]
    ls = inp["ssm_log_step"][l][:, G0:G0 + NG]
    bre = inp["ssm_b_re"][l][:, G0:G0 + NG]
    bim = inp["ssm_b_im"][l][:, G0:G0 + NG]
    cre = inp["ssm_c_re"][l][:, G0:G0 + NG]
    cim = inp["ssm_c_im"][l][:, G0:G0 + NG]
    dsk = inp["ssm_d"][l][G0 * 16:(G0 + NG) * 16]
    S = {}
    st = lambda a: np.ascontiguousarray(np.concatenate([a.reshape(NGD, 64).T] * 2, axis=0))
    S["s5_lre_s"] = st(lre)
    S["s5_lim_s"] = st(lim)
    S["s5_ls_s"] = np.ascontiguousarray(np.broadcast_to(ls.reshape(1, NGD), (128, NGD)))

    def pad_rows(a, bcast):
        out = np.zeros((4, 32, NST, 2, 64), np.float32)
        for q4 in range(4):
            for s in range(NST):
                g = min(3 * s + min(q4, 2), NG - 1)
                for d in range(2):
                    if bcast:
                        out[q4, :, s, d, :] = a[d, g][None, :]
                    elif q4 < 3:
                        out[q4, 0:16, s, d, :] = a[d, g].T
        return np.ascontiguousarray(out.reshape(128, NSL * 64))
    S["s5_lre_b"] = pad_rows(lre, True)
    S["s5_lim_b"] = pad_rows(lim, True)
    S["s5_ls_b"] = pad_rows(np.broadcast_to(ls[:, :, None], (2, NG, 64)), True)
    S["s5_bre"] = pad_rows(bre, False)
    S["s5_bim"] = pad_rows(bim, False)
    crt = cre.reshape(NGD, 16, 64).transpose(2, 0, 1)
    cit = cim.reshape(NGD, 16, 64).transpose(2, 0, 1)
    S["s5_cta"] = np.ascontiguousarray(np.concatenate([crt, cit], axis=0))
    S["s5_ctb"] = np.ascontiguousarray(np.concatenate([cit, crt], axis=0))
    dp = np.zeros((4, 32, NST), np.float32)
    for q4 in range(3):
        for s in range(NST):
            g = 3 * s + q4
            if g < NG:
                dp[q4, 0:16, s] = dsk[g * 16:(g + 1) * 16]
    S["s5_d"] = np.ascontiguousarray(dp.reshape(128, NST))
    return S


_PROG = {}


def make_in_maps(inp):
    x = inp["x"]
    meta = inp["meta_tokens"]
    ropeC, ropeS = rope_consts()
    iota_t = np.ascontiguousarray(np.broadcast_to(np.arange(LTOT, dtype=np.int16)[None, :], (128, LTOT)))
    ident = np.eye(128, dtype=np.float32)
    sel = np.zeros((8, 8, 128), np.float32)
    for e in range(8):
        sel[e, e, :] = 1.0
    shared = prep_shared(inp)
    halves = [prep_half(inp, 0), prep_half(inp, 1)]
    maps = []
    for cidx in range(NCORES):
        b, hf = cidx // 2, cidx % 2
        full = np.concatenate([meta, x[b]], axis=0)
        m = {"hT": np.ascontiguousarray(full[hf * T:(hf + 1) * T].T),
             "ropeC": np.ascontiguousarray(ropeC[:, hf * T:(hf + 1) * T]),
             "ropeS": np.ascontiguousarray(ropeS[:, hf * T:(hf + 1) * T]),
             "iota_t": iota_t, "ident": ident, "sel": sel}
        m.update(shared)
        m.update(halves[hf])
        maps.append(m)
    return maps


def kernel(**inp):
    inp = {k_: np.asarray(v) for k_, v in inp.items()}
    B = inp["x"].shape[0]
    maps = make_in_maps(inp)
    if "fused" not in _PROG:
        _PROG["fused"] = build_fused()
    res = run_bass_kernel_spmd(_PROG["fused"], maps, core_ids=list(range(NCORES)))
    hT = [res.results[c_]["hT_out"] for c_ in range(NCORES)]
    out = np.empty((B, SEQ, D), np.float32)
    for b in range(B):
        full = np.concatenate([hT[2 * b].T, hT[2 * b + 1].T], axis=0)
        out[b] = full[NMETA:]
    return out
```

```python
import math
from contextlib import ExitStack

import numpy as np
import ml_dtypes

import concourse.bass as bass
import concourse.mybir as mybir
from concourse.bass_utils import run_bass_kernel_spmd

F32 = mybir.dt.float32
BF16 = mybir.dt.bfloat16
I32 = mybir.dt.int32
I16 = mybir.dt.int16
AF = mybir.ActivationFunctionType
ALU = mybir.AluOpType
AX = mybir.AxisListType

NCORES = 8
D = 2048
DEPTH = 4
SEQ = 2048
NMETA = 16
LTOT = SEQ + NMETA
T = LTOT // 2
CH = [(0, 344), (344, 344), (688, 344)]
CHL = [(0, 512), (512, 512), (1024, 512), (1536, 512), (2048, 16)]
NKT = 17
EPS = 1e-6
HEADS = 8
NG = 32
NGD = 64
NST = 11
NSL = 2 * NST
TWO_PI_LO = 6.283185


class Tl:
    __slots__ = ("w", "r", "name")

    def __init__(self, name=""):
        self.w = None
        self.r = {}
        self.name = name


class Eng:
    def __init__(self, name, obj):
        self.name = name
        self.obj = obj
        self.semid = None
        self.cnt = 0
        self.seen = {}


class K:
    def __init__(self, nc, es):
        self.nc = nc
        self.es = es
        self.sems = []
        self.engs = {}
        for name, obj in (("pe", nc.tensor), ("dve", nc.vector), ("act", nc.scalar),
                          ("pool", nc.gpsimd), ("sp", nc.sync)):
            e = Eng(name, obj)
            e.semid = self.new_sem("e_" + name)
            self.engs[name] = e
        self.dslots = {}
        for q, n in (("sp", 24), ("pool", 24), ("act", 8)):
            self.dslots[q] = [[self.new_sem("d_%s%d" % (q, i)), 0] for i in range(n)]
        self.dnext = {"sp": 0, "pool": 0, "act": 0}
        self.psum = [es.enter_context(nc.psum_tensor("psb%d" % i, [128, 512], F32)) for i in range(8)]
        self.psum_tl = [Tl("ps%d" % i) for i in range(8)]
        self.ps_next = 0
        self.uid = 0
        self.cc_sem = self.new_sem("cc")
        self.cc_cnt = 0

    def new_sem(self, name):
        s = self.es.enter_context(self.nc.semaphore(name))
        self.sems.append(s)
        return len(self.sems) - 1

    def sb(self, es, name, shape, dt):
        self.uid += 1
        return es.enter_context(self.nc.sbuf_tensor("%s_%d" % (name, self.uid), shape, dt))

    def _wait(self, eng, reads, writes):
        deps = {}
        for t in reads:
            if t.w is not None:
                deps[t.w[0]] = max(deps.get(t.w[0], 0), t.w[1])
        for t in writes:
            if t.w is not None and t.w[0] != eng.semid:
                deps[t.w[0]] = max(deps.get(t.w[0], 0), t.w[1])
            for sid, v in t.r.items():
                if sid != eng.semid:
                    deps[sid] = max(deps.get(sid, 0), v)
        for sid, v in deps.items():
            if sid == eng.semid and eng.name == "pe":
                continue
            if eng.seen.get(sid, 0) >= v:
                continue
            eng.obj.wait_ge(self.sems[sid], v)
            eng.seen[sid] = v

    def _commit(self, tok, reads, writes):
        for t in writes:
            t.w = tok
            t.r = {}
        for t in reads:
            t.r[tok[0]] = max(t.r.get(tok[0], 0), tok[1])

    def op(self, e, reads, writes, fn):
        eng = self.engs[e]
        self._wait(eng, reads, writes)
        inst = fn(eng.obj)
        eng.cnt += 1
        inst.then_inc(self.sems[eng.semid], 1)
        self._commit((eng.semid, eng.cnt), reads, writes)

    def dma(self, q, out, in_, reads, writes, **kw):
        eng = self.engs[q]
        self._wait(eng, reads, writes)
        slots = self.dslots[q]
        i = self.dnext[q]
        self.dnext[q] = (i + 1) % len(slots)
        sid, val = slots[i]
        if val > 0 and eng.seen.get(sid, 0) < val:
            eng.obj.wait_ge(self.sems[sid], val)
            eng.seen[sid] = val
        inst = eng.obj.dma_start(out=out, in_=in_, **kw)
        inst.then_inc(self.sems[sid], 16)
        slots[i][1] = val + 16
        self._commit((sid, val + 16), reads, writes)

    def collective(self, in_t, out_t, in_tls, out_tls, groups):
        eng = self.engs["pool"]
        self._wait(eng, in_tls, out_tls)
        inst = eng.obj.collective_compute("AllGather", ALU.bypass, replica_groups=groups,
                                          ins=[in_t.ap().opt()], outs=[out_t.ap().opt()])
        self.cc_cnt += 1
        inst.then_inc(self.sems[self.cc_sem], 1)
        self._commit((self.cc_sem, self.cc_cnt), in_tls, out_tls)

    def barrier(self):
        sp = self.engs["sp"]
        for n, e in self.engs.items():
            if n != "sp" and e.cnt > 0 and sp.seen.get(e.semid, 0) < e.cnt:
                sp.obj.wait_ge(self.sems[e.semid], e.cnt)
                sp.seen[e.semid] = e.cnt
        for q in self.dslots:
            for sid, val in self.dslots[q]:
                if val > 0 and sp.seen.get(sid, 0) < val:
                    sp.obj.wait_ge(self.sems[sid], val)
                    sp.seen[sid] = val
        if self.cc_cnt > 0 and sp.seen.get(self.cc_sem, 0) < self.cc_cnt:
            sp.obj.wait_ge(self.sems[self.cc_sem], self.cc_cnt)
            sp.seen[self.cc_sem] = self.cc_cnt
        inst = sp.obj.nop()
        sp.cnt += 1
        inst.then_inc(self.sems[sp.semid], 1)
        for n, e in self.engs.items():
            if n != "sp":
                e.obj.wait_ge(self.sems[sp.semid], sp.cnt)
                e.seen[sp.semid] = sp.cnt

    def finish(self):
        self.barrier()

    def ps(self):
        i = self.ps_next
        self.ps_next = (i + 1) % 8
        return self.psum[i], self.psum_tl[i]


class Ctx:
    pass


def make_consts(k, es):
    c = Ctx()
    c.ones_f = k.sb(es, "ones_f", [128, 128], F32)
    c.ones_b = k.sb(es, "ones_b", [128, 128], BF16)
    c.tl = Tl("consts")
    k.op("dve", [], [c.tl], lambda e: e.memset(c.ones_f[:], 1.0))
    k.op("dve", [], [c.tl], lambda e: e.memset(c.ones_b[:], 1.0))
    c.eps = k.sb(es, "eps", [128, 1], F32)
    k.op("dve", [], [c.tl], lambda e: e.memset(c.eps[:], EPS))
    c.offs = k.sb(es, "offs", [128, 2], F32)
    k.op("dve", [], [c.tl], lambda e: e.memset(c.offs[:, 0:1], 0.0))
    k.op("dve", [], [c.tl], lambda e: e.memset(c.offs[:, 1:2], 0.25))
    return c


class WPool:
    def __init__(self, k, es, n, name="wslab", kt=16, mw=128):
        self.slabs = [(k.sb(es, name, [128, kt, mw], BF16), Tl(name)) for _ in range(n)]
        self.i = 0

    def get(self):
        s = self.slabs[self.i]
        self.i = (self.i + 1) % len(self.slabs)
        return s


def linear(k, wpool, w_dram, MT, KT, mw, rhs_fn, chunks, evac, q="pool"):
    for m in range(MT):
        slab, stl = wpool.get()
        k.dma(q, slab[:, 0:KT, 0:mw], w_dram[m], [], [stl])
        for ci, (n0, nsz) in enumerate(chunks):
            ps, pst = k.ps()
            for kt in range(KT):
                rap, rt = rhs_fn(kt, ci)
                k.op("pe", [stl, rt], [pst],
                     lambda e: e.matmul(ps[0:mw, 0:nsz], slab[:, kt, 0:mw], rap,
                                        start=(kt == 0), stop=(kt == KT - 1)))
            evac(m, ci, ps, pst, n0, nsz)


class NormTmp:
    def __init__(self, k, es, width=344):
        self.sq = [(k.sb(es, "sq", [128, width], BF16), Tl("sq")) for _ in range(3)]
        self.rs = [(k.sb(es, "rs", [128, width], F32), Tl("rs")) for _ in range(2)]
        self.si = 0
        self.ri = 0


def rmsnorm(k, c, nt, x, x_tl, KT, chunks, gain, gain_tl, out, out_tl, dim, out_off=0, keep=None):
    for ci, (n0, nsz) in enumerate(chunks):
        ps, pst = k.ps()
        for kt in range(KT):
            s, stl = nt.sq[nt.si % 3]
            nt.si += 1
            k.op("act", [x_tl[ci]], [stl],
                 lambda e: e.activation(s[:, 0:nsz], x[:, kt, n0:n0 + nsz], AF.Square))
            k.op("pe", [stl, c.tl], [pst],
                 lambda e: e.matmul(ps[:, 0:nsz], c.ones_b[:], s[:, 0:nsz],
                                    start=(kt == 0), stop=(kt == KT - 1)))
        if keep is None:
            r, rtl = nt.rs[nt.ri % 2]
            nt.ri += 1
            rap = r[:, 0:nsz]
        else:
            r, rtl = keep[0], keep[1][ci]
            rap = r[:, n0:n0 + nsz]
        k.op("act", [pst], [rtl],
             lambda e: e.activation(rap, ps[:, 0:nsz], AF.Sqrt, bias=c.eps[:, 0:1], scale=1.0 / dim))
        k.op("dve", [rtl], [rtl], lambda e: e.reciprocal(rap, rap))
        for kt in range(KT):
            k.op("dve", [x_tl[ci], rtl, gain_tl], [out_tl[ci]],
                 lambda e: e.scalar_tensor_tensor(out[:, out_off + kt, n0:n0 + nsz], x[:, kt, n0:n0 + nsz],
                                                  gain[:, kt:kt + 1], rap, ALU.mult, ALU.mult))


def load_small(k, es, name, dram_ap, shape, dt=F32, q="sp"):
    t = k.sb(es, name, shape, dt)
    tl = Tl(name)
    k.dma(q, t[:], dram_ap, [], [tl])
    return t, tl


def rope_evac(k, psA, psAt, psB, psBt, ropeC, ropeC_tl, ropeS, ropeS_tl, tmpA, tmpB, out_ap, out_tl, n0, nsz):
    t1, t1l = tmpA
    t2, t2l = tmpB
    k.op("dve", [psBt, ropeS_tl], [t1l],
         lambda e: e.tensor_tensor(t1[:, 0:nsz], psB[0:64, 0:nsz], ropeS[:, n0:n0 + nsz], ALU.mult))
    k.op("dve", [psAt, ropeC_tl], [t2l],
         lambda e: e.tensor_tensor(t2[:, 0:nsz], psA[0:64, 0:nsz], ropeC[:, n0:n0 + nsz], ALU.mult))
    k.op("dve", [t1l, t2l], [out_tl],
         lambda e: e.tensor_tensor(out_ap, t1[:, 0:nsz], t2[:, 0:nsz], ALU.add))


def phase_a(k, c, io, h, h_tl):
    with ExitStack() as es:
        g_mix, g_mix_tl = load_small(k, es, "g_mix", io["mix_g"], [128, 16])
        g_q, g_q_tl = load_small(k, es, "g_q", io["q_g"], [128, 4])
        g_kv, g_kv_tl = load_small(k, es, "g_kv", io["kv_g"], [128, 2])
        ropeC, ropeC_tl = load_small(k, es, "ropeC", io["ropeC"], [64, T])
        ropeS, ropeS_tl = load_small(k, es, "ropeS", io["ropeS"], [64, T])
        nt = NormTmp(k, es)
        hn = k.sb(es, "hn", [128, 16, T], BF16)
        hn_tl = [Tl("hn") for _ in CH]
        rmsnorm(k, c, nt, h, h_tl, 16, CH, g_mix, g_mix_tl, hn, hn_tl, D)
        wpool = WPool(k, es, 4)
        pj = k.sb(es, "pj", [128, 4, T], F32)
        pj_tl = [Tl("pj") for _ in CH]
        kpe = k.sb(es, "kpe", [64, T], BF16)
        kpe_tl = [Tl("kpe") for _ in CH]
        stg = [(k.sb(es, "stg", [128, 344], BF16), Tl("stg")) for _ in range(3)]
        tmpA = [(k.sb(es, "rtmp", [64, 344], F32), Tl("rtmp")) for _ in range(2)]
        tmpB = [(k.sb(es, "rtmp2", [64, 344], F32), Tl("rtmp2")) for _ in range(2)]
        cqn = k.sb(es, "cqn", [128, 4, T], BF16)
        cqn_tl = [Tl("cqn") for _ in CH]
        ckvn = k.sb(es, "ckvn", [128, 2, T], BF16)
        ckvn_tl = [Tl("ckvn") for _ in CH]
        w = io["w_in_t"]

        def rhs(kt, ci):
            n0, nsz = CH[ci]
            return hn[:, kt, n0:n0 + nsz], hn_tl[ci]

        def evac_pj(off):
            def f(m, ci, ps, pst, n0, nsz):
                if (m + ci) % 2 == 0:
                    k.op("act", [pst], [pj_tl[ci]], lambda e: e.copy(pj[:, m, n0:n0 + nsz], ps[:, 0:nsz]))
                else:
                    k.op("dve", [pst], [pj_tl[ci]], lambda e: e.tensor_copy(pj[:, m, n0:n0 + nsz], ps[:, 0:nsz]))
            return f

        linear(k, wpool, w[0:4], 4, 16, 128, rhs, CH, evac_pj(0))
        rmsnorm(k, c, nt, pj, pj_tl, 4, CH, g_q, g_q_tl, cqn, cqn_tl, 512)
        for kt in range(4):
            k.dma("sp", io["cq_n"][kt * 128:(kt + 1) * 128, :], cqn[:, kt, :], cqn_tl, io["cq_n_tl"])
        linear(k, wpool, w[4:6], 2, 16, 128, rhs, CH, evac_pj(0))
        rmsnorm(k, c, nt, pj, pj_tl, 2, CH, g_kv, g_kv_tl, ckvn, ckvn_tl, 256)
        for kt in range(2):
            k.dma("sp", io["kvx"][kt * 128:(kt + 1) * 128, :], ckvn[:, kt, :], ckvn_tl, io["kvx_tl"])
        slab, stl = wpool.get()
        k.dma("pool", slab[:, 0:16, :], w[6], [], [stl])
        for ci, (n0, nsz) in enumerate(CH):
            psA, psAt = k.ps()
            psB, psBt = k.ps()
            for kt in range(16):
                k.op("pe", [stl, hn_tl[ci]], [psAt],
                     lambda e: e.matmul(psA[0:64, 0:nsz], slab[:, kt, 0:64], hn[:, kt, n0:n0 + nsz],
                                        start=(kt == 0), stop=(kt == 15)))
            for kt in range(16):
                k.op("pe", [stl, hn_tl[ci]], [psBt],
                     lambda e: e.matmul(psB[0:64, 0:nsz], slab[:, kt, 64:128], hn[:, kt, n0:n0 + nsz],
                                        start=(kt == 0), stop=(kt == 15)))
            rope_evac(k, psA, psAt, psB, psBt, ropeC, ropeC_tl, ropeS, ropeS_tl, tmpA[ci % 2], tmpB[ci % 2],
                      kpe[:, n0:n0 + nsz], kpe_tl[ci], n0, nsz)
        k.dma("sp", io["kvx"][256:320, :], kpe[:, :], kpe_tl, io["kvx_tl"])
        cnt = [0]

        def evac_u(m, ci, ps, pst, n0, nsz):
            s, sl = stg[cnt[0] % 3]
            cnt[0] += 1
            if cnt[0] % 2 == 0:
                k.op("act", [pst], [sl], lambda e: e.copy(s[:, 0:nsz], ps[:, 0:nsz]))
            else:
                k.op("dve", [pst], [sl], lambda e: e.tensor_copy(s[:, 0:nsz], ps[:, 0:nsz]))
            dst, dtl = io["u_dst"](m)
            k.dma("sp", dst[:, n0:n0 + nsz], s[:, 0:nsz], [sl], dtl)

        linear(k, wpool, w[7:15], 8, 16, 128, rhs, CH, evac_u)
        k.barrier()


def phase_attn(k, c, io):
    scale = 192.0 ** -0.5
    with ExitStack() as es:
        cqn = k.sb(es, "cqn", [128, 4, T], BF16)
        cqn_tl = Tl("cqn")
        for kt in range(4):
            k.dma("sp", cqn[:, kt, :], io["cq_n"][kt * 128:(kt + 1) * 128, :], io["cq_n_tl"], [cqn_tl])
        kvn = k.sb(es, "kvn", [128, 2, LTOT], BF16)
        kvn_tl = Tl("kvn")
        for kt in range(2):
            for hh in range(2):
                src_, stl_ = io["kvx_half"](hh)
                k.dma("sp", kvn[:, kt, hh * T:(hh + 1) * T], src_[kt * 128:(kt + 1) * 128, :], stl_, [kvn_tl])
        kpe = k.sb(es, "kpe", [64, LTOT], BF16)
        kpe_tl = Tl("kpe")
        for hh in range(2):
            src_, stl_ = io["kvx_half"](hh)
            k.dma("sp", kpe[:, hh * T:(hh + 1) * T], src_[256:320, :], stl_, [kpe_tl])
        g_att, g_att_tl = load_small(k, es, "g_att", io["att_g"], [128, 8])
        ropeC, ropeC_tl = load_small(k, es, "ropeC", io["ropeC"], [64, T])
        ropeS, ropeS_tl = load_small(k, es, "ropeS", io["ropeS"], [64, T])
        wv = k.sb(es, "wv", [128, 2, 1024], BF16)
        wv_tl = Tl("wv")
        k.dma("pool", wv[:, 0, :], io["w_v"][:, 0, :], [], [wv_tl])
        k.dma("pool", wv[:, 1, :], io["w_v"][:, 1, :], [], [wv_tl])
        wpool = WPool(k, es, 4, kt=4)
        nt = NormTmp(k, es)
        HG = 2
        qn = k.sb(es, "qn", [128, HG, T], BF16)
        qn_tl = [Tl("qn") for _ in range(HG)]
        qr = k.sb(es, "qr", [64, HG, T], BF16)
        qr_tl = [Tl("qr") for _ in range(HG)]
        kn = k.sb(es, "kn", [128, HG, LTOT], BF16)
        kn_tl = [Tl("kn") for _ in range(HG)]
        vall = k.sb(es, "vall", [128, NKT, HG * 128], BF16)
        vall_tl = Tl("vall")
        att = k.sb(es, "att", [128, 8, T], F32)
        att_tl = [Tl("att") for _ in CH]
        tmpA = [(k.sb(es, "rtmp", [64, 344], F32), Tl("rtmp")) for _ in range(2)]
        tmpB = [(k.sb(es, "rtmp2", [64, 344], F32), Tl("rtmp2")) for _ in range(2)]
        pT = [(k.sb(es, "pT", [128, 344], BF16), Tl("pT")) for _ in range(3)]
        rden = [(k.sb(es, "rden", [128, 344], F32), Tl("rden")) for _ in range(2)]
        s_banks = [0, 1, 2]
        acc_banks = [(3, 4), (5, 6)]
        it = 0
        si_box = [0]
        for hg in range(8 // HG):
            for j in range(HG):
                hd = hg * HG + j
                slab, stl = wpool.get()
                k.dma("pool", slab[:, 0:4, :], io["w_uq_t"][2 * hd], [], [stl])
                for ci, (n0, nsz) in enumerate(CH):
                    ps, pst = k.psum[7], k.psum_tl[7]
                    for kt in range(4):
                        k.op("pe", [stl, cqn_tl], [pst],
                             lambda e: e.matmul(ps[:, 0:nsz], slab[:, kt, :], cqn[:, kt, n0:n0 + nsz],
                                                start=(kt == 0), stop=(kt == 3)))
                    k.op("act", [pst], [qn_tl[j]], lambda e: e.copy(qn[:, j, n0:n0 + nsz], ps[:, 0:nsz]))
                slab, stl = wpool.get()
                k.dma("pool", slab[:, 0:4, :], io["w_uq_t"][2 * hd + 1], [], [stl])
                for ci, (n0, nsz) in enumerate(CH):
                    psA, psAt = k.psum[7], k.psum_tl[7]
                    psB, psBt = k.psum[0], k.psum_tl[0]
                    for kt in range(4):
                        k.op("pe", [stl, cqn_tl], [psAt],
                             lambda e: e.matmul(psA[0:64, 0:nsz], slab[:, kt, 0:64], cqn[:, kt, n0:n0 + nsz],
                                                start=(kt == 0), stop=(kt == 3)))
                    for kt in range(4):
                        k.op("pe", [stl, cqn_tl], [psBt],
                             lambda e: e.matmul(psB[0:64, 0:nsz], slab[:, kt, 64:128], cqn[:, kt, n0:n0 + nsz],
                                                start=(kt == 0), stop=(kt == 3)))
                    rope_evac(k, psA, psAt, psB, psBt, ropeC, ropeC_tl, ropeS, ropeS_tl, tmpA[ci % 2], tmpB[ci % 2],
                              qr[:, j, n0:n0 + nsz], qr_tl[j], n0, nsz)
            for j in range(HG):
                hd = hg * HG + j
                slab, stl = wpool.get()
                k.dma("pool", slab[:, 0:2, :], io["w_uk_t"][hd], [], [stl])
                for ci, (n0, nsz) in enumerate(CHL):
                    bb = 7 if ci % 2 == 0 else 0
                    ps, pst = k.psum[bb], k.psum_tl[bb]
                    for kt in range(2):
                        k.op("pe", [stl, kvn_tl], [pst],
                             lambda e: e.matmul(ps[:, 0:nsz], slab[:, kt, :], kvn[:, kt, n0:n0 + nsz],
                                                start=(kt == 0), stop=(kt == 1)))
                    if ci % 2 == 0:
                        k.op("act", [pst], [kn_tl[j]], lambda e: e.copy(kn[:, j, n0:n0 + nsz], ps[:, 0:nsz]))
                    else:
                        k.op("dve", [pst], [kn_tl[j]], lambda e: e.tensor_copy(kn[:, j, n0:n0 + nsz], ps[:, 0:nsz]))
            VW = HG * 128
            for kt_ in range(NKT):
                k0 = kt_ * 128
                ksz = min(128, LTOT - k0)
                bb = 7 if kt_ % 2 == 0 else 0
                ps, pst = k.psum[bb], k.psum_tl[bb]
                for j2 in range(2):
                    k.op("pe", [kvn_tl, wv_tl], [pst],
                         lambda e: e.matmul(ps[0:ksz, 0:VW], kvn[:, j2, k0:k0 + ksz], wv[:, j2, hg * VW:(hg + 1) * VW],
                                            start=(j2 == 0), stop=(j2 == 1)))
                if kt_ % 2 == 0:
                    k.op("act", [pst], [vall_tl], lambda e: e.copy(vall[0:ksz, kt_, :], ps[0:ksz, 0:VW]))
                else:
                    k.op("dve", [pst], [vall_tl], lambda e: e.tensor_copy(vall[0:ksz, kt_, :], ps[0:ksz, 0:VW]))
            for j in range(HG):
                hd = hg * HG + j
                for ci, (n0, nsz) in enumerate(CH):
                    ob, db = acc_banks[it % 2]
                    po, pot = k.psum[ob], k.psum_tl[ob]
                    pd, pdt = k.psum[db], k.psum_tl[db]
                    def emit_s(kt_):
                        nonlocal_si = si_box[0]
                        k0 = kt_ * 128
                        ksz = min(128, LTOT - k0)
                        sb_ = s_banks[nonlocal_si % 3]
                        psc, psct = k.psum[sb_], k.psum_tl[sb_]
                        p, ptl = pT[nonlocal_si % 3]
                        si_box[0] += 1
                        k.op("pe", [kn_tl[j], qn_tl[j]], [psct],
                             lambda e: e.matmul(psc[0:ksz, 0:nsz], kn[:, j, k0:k0 + ksz], qn[:, j, n0:n0 + nsz],
                                                start=True, stop=False))
                        k.op("pe", [kpe_tl, qr_tl[j]], [psct],
                             lambda e: e.matmul(psc[0:ksz, 0:nsz], kpe[:, k0:k0 + ksz], qr[:, j, n0:n0 + nsz],
                                                start=False, stop=True))
                        k.op("act", [psct], [ptl],
                             lambda e: e.activation(p[0:ksz, 0:nsz], psc[0:ksz, 0:nsz], AF.Exp, scale=scale))
                        return (kt_, ksz, p, ptl)

                    def emit_pv(st):
                        kt_, ksz, p, ptl = st
                        k.op("pe", [ptl, vall_tl], [pot],
                             lambda e: e.matmul(po[:, 0:nsz], vall[0:ksz, kt_, j * 128:(j + 1) * 128], p[0:ksz, 0:nsz],
                                                start=(kt_ == 0), stop=(kt_ == NKT - 1)))
                        k.op("pe", [ptl, c.tl], [pdt],
                             lambda e: e.matmul(pd[:, 0:nsz], c.ones_b[0:ksz, :], p[0:ksz, 0:nsz],
                                                start=(kt_ == 0), stop=(kt_ == NKT - 1)))

                    pend = [emit_s(0)]
                    for kt_ in range(1, NKT):
                        pend.append(emit_s(kt_))
                        emit_pv(pend.pop(0))
                    emit_pv(pend.pop(0))
                    r, rtl = rden[it % 2]
                    k.op("dve", [pdt], [rtl], lambda e: e.reciprocal(r[:, 0:nsz], pd[:, 0:nsz]))
                    k.op("dve", [pot, rtl], [att_tl[ci]],
                         lambda e: e.tensor_tensor(att[:, hd, n0:n0 + nsz], po[:, 0:nsz], r[:, 0:nsz], ALU.mult))
                    it += 1
        k.ps_next = 0
        attn = k.sb(es, "attn", [128, 8, T], BF16)
        attn_tl = [Tl("attn") for _ in CH]
        rmsnorm(k, c, nt, att, att_tl, 8, CH, g_att, g_att_tl, attn, attn_tl, 1024)
        for kt in range(8):
            k.dma("sp", io["att_n"][kt * 128:(kt + 1) * 128, :], attn[:, kt, :], attn_tl, io["att_n_tl"])
        k.barrier()


def phase_s5(k, c, io):
    NP = NSL * 64
    with ExitStack() as es:
        rr = k.sb(es, "rr", [128, NGD], F32)
        phi = k.sb(es, "phi", [128, NGD], F32)
        sp_tl = Tl("s5par")
        lB = k.sb(es, "lB", [128, NSL, 128], BF16)
        lBs = k.sb(es, "lBs", [128, NSL, 128], BF16)
        lB_tl = Tl("lB")
        l1 = k.sb(es, "l1", [128, NGD, 32], BF16)
        l2 = k.sb(es, "l2", [128, NGD, 32], BF16)
        l12_tl = Tl("l12")
        with ExitStack() as ep:
            lre, lre_tl = load_small(k, ep, "lre", io["s5_lre_s"], [128, NGD])
            lim, lim_tl = load_small(k, ep, "lim", io["s5_lim_s"], [128, NGD])
            lst, lst_tl = load_small(k, ep, "lst", io["s5_ls_s"], [128, NGD])
            dt_s = k.sb(ep, "dt_s", [128, NGD], F32)
            dt_tl = Tl("dt_s")
            k.op("act", [lst_tl], [dt_tl], lambda e: e.activation(dt_s[:], lst[:], AF.Exp))
            k.op("dve", [lre_tl, dt_tl], [sp_tl], lambda e: e.tensor_tensor(rr[:], lre[:], dt_s[:], ALU.mult))
            k.op("act", [sp_tl], [sp_tl], lambda e: e.activation(rr[:], rr[:], AF.Exp))
            k.op("dve", [lim_tl, dt_tl, sp_tl], [sp_tl], lambda e: e.tensor_tensor(phi[:], lim[:], dt_s[:], ALU.mult))
            k.op("dve", [sp_tl], [sp_tl],
                 lambda e: e.tensor_scalar(phi[:], phi[:], 1.0 / (2.0 * math.pi), None, ALU.mult))
            lreb, lreb_tl = load_small(k, ep, "lreb", io["s5_lre_b"], [128, NP])
            limb, limb_tl = load_small(k, ep, "limb", io["s5_lim_b"], [128, NP])
            lsb, lsb_tl = load_small(k, ep, "lsb", io["s5_ls_b"], [128, NP])
            bre, bre_tl = load_small(k, ep, "bre", io["s5_bre"], [128, NP])
            bim, bim_tl = load_small(k, ep, "bim", io["s5_bim"], [128, NP])
            X = {}
            xt = Tl("s5tmp")
            for nm in ("dt", "mag", "tu", "fr", "sn", "cs", "ar", "ai", "den", "kr", "ki", "t1", "t2"):
                X[nm] = k.sb(ep, "x_" + nm, [128, NP], F32)
            ki32 = k.sb(ep, "ki32", [128, NP], I32)
            ins = [xt, lreb_tl, limb_tl, lsb_tl, bre_tl, bim_tl]

            def dv(fn):
                k.op("dve", ins, [xt], fn)

            def ac(fn):
                k.op("act", ins, [xt], fn)

            ac(lambda e: e.activation(X["dt"][:], lsb[:], AF.Exp))
            dv(lambda e: e.tensor_tensor(X["mag"][:], lreb[:], X["dt"][:], ALU.mult))
            ac(lambda e: e.activation(X["mag"][:], X["mag"][:], AF.Exp))
            dv(lambda e: e.tensor_tensor(X["tu"][:], limb[:], X["dt"][:], ALU.mult))
            dv(lambda e: e.tensor_scalar(X["tu"][:], X["tu"][:], 1.0 / (2.0 * math.pi), None, ALU.mult))

            def sin_turns(dst, src, off):
                if off != 0.0:
                    dv(lambda e: e.tensor_scalar(X["t1"][:], src[:], off, None, ALU.add))
                    s2 = X["t1"]
                else:
                    s2 = src
                dv(lambda e: e.tensor_copy(ki32[:], s2[:]))
                dv(lambda e: e.tensor_tensor(X["fr"][:], s2[:], ki32[:], ALU.subtract))
                ac(lambda e: e.activation(dst[:], X["fr"][:], AF.Sin, scale=TWO_PI_LO))

            sin_turns(X["sn"], X["tu"], 0.0)
            sin_turns(X["cs"], X["tu"], 0.25)
            dv(lambda e: e.tensor_tensor(X["ar"][:], X["mag"][:], X["cs"][:], ALU.mult))
            dv(lambda e: e.tensor_scalar(X["ar"][:], X["ar"][:], -1.0, None, ALU.add))
            dv(lambda e: e.tensor_tensor(X["ai"][:], X["mag"][:], X["sn"][:], ALU.mult))
            dv(lambda e: e.tensor_tensor(X["den"][:], lreb[:], lreb[:], ALU.mult))
            dv(lambda e: e.tensor_tensor(X["t1"][:], limb[:], limb[:], ALU.mult))
            dv(lambda e: e.tensor_tensor(X["den"][:], X["den"][:], X["t1"][:], ALU.add))
            dv(lambda e: e.reciprocal(X["den"][:], X["den"][:]))
            dv(lambda e: e.tensor_tensor(X["kr"][:], X["ar"][:], lreb[:], ALU.mult))
            dv(lambda e: e.tensor_tensor(X["t1"][:], X["ai"][:], limb[:], ALU.mult))
            dv(lambda e: e.tensor_tensor(X["kr"][:], X["kr"][:], X["t1"][:], ALU.add))
            dv(lambda e: e.tensor_tensor(X["kr"][:], X["kr"][:], X["den"][:], ALU.mult))
            dv(lambda e: e.tensor_tensor(X["ki"][:], X["ai"][:], lreb[:], ALU.mult))
            dv(lambda e: e.tensor_tensor(X["t1"][:], X["ar"][:], limb[:], ALU.mult))
            dv(lambda e: e.tensor_tensor(X["ki"][:], X["ki"][:], X["t1"][:], ALU.subtract))
            dv(lambda e: e.tensor_tensor(X["ki"][:], X["ki"][:], X["den"][:], ALU.mult))
            dv(lambda e: e.tensor_tensor(X["t1"][:], X["kr"][:], bre[:], ALU.mult))
            dv(lambda e: e.tensor_tensor(X["t2"][:], X["ki"][:], bim[:], ALU.mult))
            dv(lambda e: e.tensor_tensor(X["ar"][:], X["t1"][:], X["t2"][:], ALU.subtract))
            dv(lambda e: e.tensor_tensor(X["t1"][:], X["kr"][:], bim[:], ALU.mult))
            dv(lambda e: e.tensor_tensor(X["t2"][:], X["ki"][:], bre[:], ALU.mult))
            dv(lambda e: e.tensor_tensor(X["ai"][:], X["t1"][:], X["t2"][:], ALU.add))
            v3 = lambda a: a[:].rearrange("c (g p) -> c g p", p=64)
            k.op("dve", [xt], [lB_tl], lambda e: e.tensor_copy(lB[:, :, 0:64], v3(X["ar"])))
            k.op("dve", [xt], [lB_tl], lambda e: e.tensor_copy(lB[:, :, 64:128], v3(X["ai"])))
            k.op("dve", [xt], [lB_tl], lambda e: e.tensor_copy(lBs[:, :, 0:64], v3(X["ai"])))
            k.op("dve", [xt], [lB_tl], lambda e: e.tensor_scalar(lBs[:, :, 64:128], v3(X["ar"]), -1.0, None, ALU.mult))
            cta, cta_tl = load_small(k, ep, "cta", io["s5_cta"], [128, NGD, 16])
            ctb, ctb_tl = load_small(k, ep, "ctb", io["s5_ctb"], [128, NGD, 16])
            k.op("pool", [], [l12_tl], lambda e: e.memset(l1[:], 0.0))
            k.op("pool", [], [l12_tl], lambda e: e.memset(l2[:], 0.0))
            k.op("dve", [cta_tl, l12_tl], [l12_tl], lambda e: e.tensor_copy(l1[0:64, :, 0:16], cta[0:64]))
            k.op("dve", [cta_tl, l12_tl], [l12_tl],
                 lambda e: e.tensor_scalar(l1[64:128, :, 0:16], cta[64:128], -1.0, None, ALU.mult))
            k.op("dve", [ctb_tl, l12_tl], [l12_tl],
                 lambda e: e.tensor_scalar(l2[:, :, 0:16], ctb[:], -1.0, None, ALU.mult))
            k.barrier()
        Ubuf = [(k.sb(es, "U", [128, LTOT], BF16), Tl("U")) for _ in range(2)]
        Ua, Ua_tl = k.sb(es, "Ua", [128, LTOT], BF16), Tl("Ua")
        Ub, Ub_tl = k.sb(es, "Ub", [128, LTOT], BF16), Tl("Ub")
        msk, msk_tl = load_small(k, es, "msk", io["msk"], [128, 2])
        for ub_, ubl_ in Ubuf + [(Ua, Ua_tl), (Ub, Ub_tl)]:
            k.op("pool", [], [ubl_], lambda e: e.memset(ub_[:], 0.0))
        dsk, dsk_tl = load_small(k, es, "dsk", io["s5_d"], [128, NST])
        iot, iot_tl = load_small(k, es, "iot", io["iota_t"], [128, LTOT], dt=I16)
        gout = [(k.sb(es, "gout", [128, LTOT], F32), Tl("gout")) for _ in range(1)]
        gsel = [k.sb(es, "gsel", [128, T], F32) for _ in range(2)]
        gsel_tl = [Tl("gsel") for _ in range(2)]

        def mk(nm, dt, n):
            return [(k.sb(es, nm, [128, LTOT], dt), Tl(nm)) for _ in range(n)]
        tau = mk("tau", F32, 1) * 2
        gtmp, gtmp_tl = tau[0]
        kin = mk("kin", I16, 2)
        COS = mk("COS", BF16, 3)
        SIN = mk("SIN", BF16, 3)
        wbuf = mk("wbuf", BF16, 2)
        qbuf = mk("qbuf", BF16, 2)
        t1b = mk("t1b", BF16, 2)
        E1 = mk("E1", BF16, 2)
        E2 = mk("E2", BF16, 2)
        ybank = [4, 5, 6, 7, 3]

        def load_U(s):
            U, U_tl = Ubuf[s % 2]
            for q4 in range(3):
                gl = s * 3 + q4
                if gl < NG:
                    for hh in range(2):
                        k.dma("sp", Ua[32 * q4:32 * q4 + 16, hh * T:(hh + 1) * T],
                              io["u_loc"][gl * 16:(gl + 1) * 16, :], io["u_loc_tl"], [Ua_tl])
                        src_, stl_ = io["u_par"](hh)
                        k.dma("sp", Ub[32 * q4:32 * q4 + 16, hh * T:(hh + 1) * T], src_[gl * 16:(gl + 1) * 16, :], stl_, [Ub_tl])
            for hh in range(2):
                cs_ = slice(hh * T, (hh + 1) * T)
                k.op("dve", [Ua_tl, msk_tl], [Ua_tl],
                     lambda e: e.tensor_scalar(Ua[:, cs_], Ua[:, cs_], msk[:, hh:hh + 1], None, ALU.mult))
                k.op("dve", [Ua_tl, Ub_tl, msk_tl], [U_tl],
                     lambda e: e.scalar_tensor_tensor(U[:, cs_], Ub[:, cs_], msk[:, 1 - hh:2 - hh], Ua[:, cs_], ALU.mult, ALU.add))

        def stage_T(g, part):
            gd, i3 = g["gd"], g["idx"] % 3
            ta, tal = tau[0]

            def head(ti):
                ka, kal = kin[ti]
                k.op("act", [iot_tl, sp_tl], [tal],
                     lambda e: e.activation(ta[:], iot[:], AF.Identity, scale=phi[:, gd:gd + 1], bias=c.offs[:, ti:ti + 1]))
                k.op("act", [tal], [kal], lambda e: e.activation(ka[:], ta[:], AF.Identity))

            def tail(ti, dst):
                ka, kal = kin[ti]
                k.op("dve", [tal, kal], [tal], lambda e: e.tensor_tensor(ta[:], ta[:], ka[:], ALU.subtract))
                k.op("act", [tal], [dst[1]],
                     lambda e: e.activation(dst[0][:], ta[:], AF.Sin, scale=TWO_PI_LO))
            if part == 0:
                head(0)
                tail(0, SIN[i3])
                head(1)
            else:
                tail(1, COS[i3])

        def stage_I(g):
            s, q4, d, sd, i2 = g["s"], g["q4"], g["d"], g["sd"], g["idx"] % 2
            U, U_tl = Ubuf[s % 2]
            cs, cstl = COS[g["idx"] % 3]
            sn, sntl = SIN[g["idx"] % 3]
            w_, wtl = wbuf[i2]
            t1_, t1tl = t1b[i2]
            for ci, (n0, nsz) in enumerate(CHL):
                bA, bB = (2 * ci) % 3, (2 * ci + 1) % 3
                psA, psAt = k.psum[bA], k.psum_tl[bA]
                psB, psBt = k.psum[bB], k.psum_tl[bB]
                k.op("pe", [lB_tl, U_tl], [psAt],
                     lambda e: e.matmul(psA[:, 0:nsz], lB[32 * q4:32 * q4 + 16, sd, :],
                                        U[32 * q4:32 * q4 + 16, n0:n0 + nsz], start=True, stop=True))
                k.op("pe", [lB_tl, U_tl], [psBt],
                     lambda e: e.matmul(psB[:, 0:nsz], lBs[32 * q4:32 * q4 + 16, sd, :],
                                        U[32 * q4:32 * q4 + 16, n0:n0 + nsz], start=True, stop=True))
                if d == 0:
                    so = slice(n0, n0 + nsz)
                    inB = psB[:, 0:nsz]
                    outA = w_[:, so]
                else:
                    s0 = LTOT - n0 - nsz
                    so = slice(s0, s0 + nsz)
                    inB = psB[:, 0:nsz][:, ::-1]
                    outA = w_[:, so][:, ::-1]
                k.op("act", [psAt], [wtl], lambda e: e.copy(outA, psA[:, 0:nsz]))
                k.op("dve", [psBt, sntl], [t1tl],
                     lambda e: e.tensor_tensor(t1_[:, so], inB, sn[:, so], ALU.mult))
            k.op("dve", [wtl, cstl], [wtl], lambda e: e.tensor_tensor(w_[:], w_[:], cs[:], ALU.mult))
            k.op("dve", [t1tl, wtl], [wtl], lambda e: e.tensor_tensor(w_[:], w_[:], t1_[:], ALU.add))

        def stage_S(g):
            gd, i2 = g["gd"], g["idx"] % 2
            w_, wtl = wbuf[i2]
            qb, qtl = qbuf[i2]
            k.op("dve", [wtl, sp_tl], [qtl],
                 lambda e: e.tensor_tensor_scan(qb[:], rr[:, gd:gd + 1].broadcast_to([128, LTOT]), w_[:], 0.0,
                                                ALU.mult, ALU.add))

        def stage_O(g):
            q4, d, gd, i2 = g["q4"], g["d"], g["gd"], g["idx"] % 2
            cs, cstl = COS[g["idx"] % 3]
            sn, sntl = SIN[g["idx"] % 3]
            qb, qtl = qbuf[i2]
            e1, e1tl = E1[i2]
            e2, e2tl = E2[i2]
            o1 = e1[:, ::-1] if d == 1 else e1[:]
            o2 = e2[:, ::-1] if d == 1 else e2[:]
            k.op("dve", [qtl, cstl], [e1tl], lambda e: e.tensor_tensor(o1, qb[:], cs[:], ALU.mult))
            k.op("dve", [qtl, sntl], [e2tl], lambda e: e.tensor_tensor(o2, qb[:], sn[:], ALU.mult))
            for ci, (n0, nsz) in enumerate(CHL):
                py, pyt = k.psum[ybank[ci]], k.psum_tl[ybank[ci]]
                oap = py[32 * q4:32 * q4 + 32, 0:nsz]
                k.op("pe", [l12_tl, e1tl], [pyt],
                     lambda e: e.matmul(oap, l1[:, gd, :], e1[:, n0:n0 + nsz], start=(d == 0), stop=False))
                k.op("pe", [l12_tl, e2tl], [pyt],
                     lambda e: e.matmul(oap, l2[:, gd, :], e2[:, n0:n0 + nsz], start=False, stop=(d == 1)))

        def evac_tile(s):
            U, U_tl = Ubuf[s % 2]
            go, gotl = gout[0]
            PV = 96 if s < NST - 1 else 64
            for ci, (n0, nsz) in enumerate(CHL):
                py, pyt = k.psum[ybank[ci]], k.psum_tl[ybank[ci]]
                k.op("dve", [pyt, U_tl, dsk_tl], [gotl],
                     lambda e: e.scalar_tensor_tensor(go[0:PV, n0:n0 + nsz], U[0:PV, n0:n0 + nsz], dsk[0:PV, s:s + 1],
                                                      py[0:PV, 0:nsz], ALU.mult, ALU.add))
            k.op("act", [gotl], [gtmp_tl], lambda e: e.activation(gtmp[0:PV], go[0:PV], AF.Square))
            k.op("dve", [gtmp_tl], [gtmp_tl],
                 lambda e: e.tensor_scalar(gtmp[0:PV], gtmp[0:PV], 0.044715, 1.0, ALU.mult, ALU.add))
            k.op("dve", [gtmp_tl, gotl], [gtmp_tl], lambda e: e.tensor_tensor(gtmp[0:PV], gtmp[0:PV], go[0:PV], ALU.mult))
            k.op("act", [gtmp_tl], [gtmp_tl], lambda e: e.activation(gtmp[0:PV], gtmp[0:PV], AF.Sigmoid, scale=1.5957691216))
            k.op("dve", [gtmp_tl, gotl], [gotl], lambda e: e.tensor_tensor(go[0:PV], go[0:PV], gtmp[0:PV], ALU.mult))
            g0, g1 = go[0:PV, 0:T], go[0:PV, T:2 * T]
            k.op("dve", [gotl, msk_tl], [gsel_tl[0]],
                 lambda e: e.tensor_scalar(gsel[0][0:PV, :], g0, msk[0:PV, 0:1], None, ALU.mult))
            k.op("dve", [gotl, msk_tl, gsel_tl[0]], [gsel_tl[0]],
                 lambda e: e.scalar_tensor_tensor(gsel[0][0:PV, :], g1, msk[0:PV, 1:2], gsel[0][0:PV, :], ALU.mult, ALU.add))
            k.op("dve", [gotl, msk_tl], [gsel_tl[1]],
                 lambda e: e.tensor_scalar(gsel[1][0:PV, :], g0, msk[0:PV, 1:2], None, ALU.mult))
            k.op("dve", [gotl, msk_tl, gsel_tl[1]], [gsel_tl[1]],
                 lambda e: e.scalar_tensor_tensor(gsel[1][0:PV, :], g1, msk[0:PV, 0:1], gsel[1][0:PV, :], ALU.mult, ALU.add))
            for q4 in range(3):
                gl = s * 3 + q4
                if gl < NG:
                    for which in range(2):
                        dst, dtl = io["g_dst"](which, gl)
                        k.dma("sp", dst, gsel[which][32 * q4:32 * q4 + 16, :], [gsel_tl[which]], dtl)

        gds = []
        for s in range(NST):
            tile_g = [(q4, d) for q4 in range(3) if s * 3 + q4 < NG for d in range(2)]
            for j, (q4, d) in enumerate(tile_g):
                gl = s * 3 + q4
                gds.append({"s": s, "q4": q4, "d": d, "gd": d * NG + gl, "sd": s * 2 + d, "idx": len(gds),
                            "first": j == 0, "last": j == len(tile_g) - 1})
        load_U(0)
        stage_T(gds[0], 0)
        stage_T(gds[0], 1)
        for i, g in enumerate(gds):
            stage_I(g)
            if i + 1 < len(gds):
                stage_T(gds[i + 1], 0)
            if i >= 1:
                pg = gds[i - 1]
                stage_S(pg)
                stage_O(pg)
            if i + 1 < len(gds):
                stage_T(gds[i + 1], 1)
            if i >= 1 and gds[i - 1]["last"]:
                evac_tile(gds[i - 1]["s"])
            if g["first"] and g["s"] + 1 < NST:
                load_U(g["s"] + 1)
        pg = gds[-1]
        stage_S(pg)
        stage_O(pg)
        evac_tile(pg["s"])
        k.ps_next = 0
        k.barrier()


def moe_gates(k, c, es, io, h, h_tl, g_ffn, g_ffn_tl, rsk, rsk_tl):
    wr, wr_tl = load_small(k, es, "wr", io["router"], [128, 16, 8])
    ident, ident_tl = load_small(k, es, "ident", io["ident"], [128, 128])
    sel, sel_tl = load_small(k, es, "sel", io["sel"], [8, 8, 128])
    k.op("dve", [wr_tl, g_ffn_tl], [wr_tl],
         lambda e: e.tensor_tensor(wr[:], wr[:], g_ffn[:].unsqueeze(2).broadcast_to([128, 16, 8]), ALU.mult))
    GT = k.sb(es, "GT", [8, T], F32)
    GT_tl = Tl("GT")
    W = {}
    wl = Tl("gw")
    for nm, wd in (("lg", 16), ("lgs", 8), ("m8", 8), ("mk", 8), ("ntp", 1), ("ex", 8), ("em", 8), ("dn", 1), ("gt", 8)):
        W[nm] = k.sb(es, "gw_" + nm, [128, wd], F32)
    ntile = (T + 127) // 128
    for tt in range(ntile):
        t0 = tt * 128
        tsz = min(128, T - t0)
        ci = next(i for i, (n0, nsz) in enumerate(CH) if n0 <= t0 < n0 + nsz)
        ci2 = next(i for i, (n0, nsz) in enumerate(CH) if n0 <= t0 + tsz - 1 < n0 + nsz)
        deps_h = [h_tl[ci]] + ([h_tl[ci2]] if ci2 != ci else [])
        deps_r = [rsk_tl[ci]] + ([rsk_tl[ci2]] if ci2 != ci else [])
        ps, pst = k.ps()
        for kt in range(16):
            k.op("pe", deps_h + [wr_tl], [pst],
                 lambda e: e.matmul(ps[0:tsz, 0:8], h[:, kt, t0:t0 + tsz], wr[:, kt, :],
                                    start=(kt == 0), stop=(kt == 15)))
        ps2, ps2t = k.ps()
        k.op("pe", deps_r + [c.tl], [ps2t],
             lambda e: e.matmul(ps2[0:tsz, 0:2], rsk[0:1, t0:t0 + tsz], c.ones_f[0:1, 0:2], start=True, stop=True))
        k.op("dve", [pst, wl], [wl], lambda e: e.tensor_copy(W["lg"][0:tsz, 0:8], ps[0:tsz, 0:8]))
        k.op("dve", [ps2t, wl], [wl], lambda e: e.tensor_copy(W["lg"][0:tsz, 8:10], ps2[0:tsz, 0:2]))
        k.op("dve", [wl], [wl],
             lambda e: e.tensor_scalar(W["lgs"][0:tsz, :], W["lg"][0:tsz, 0:8], W["lg"][0:tsz, 8:9], None, ALU.mult))
        k.op("dve", [wl], [wl], lambda e: e.max(W["m8"][0:tsz, :], W["lgs"][0:tsz, :]))
        k.op("dve", [wl], [wl],
             lambda e: e.tensor_scalar(W["mk"][0:tsz, :], W["lgs"][0:tsz, :], W["m8"][0:tsz, 1:2], None, ALU.is_ge))
        k.op("dve", [wl], [wl],
             lambda e: e.tensor_scalar(W["ntp"][0:tsz, :], W["m8"][0:tsz, 0:1], -1.0, None, ALU.mult))
        k.op("act", [wl], [wl],
             lambda e: e.activation(W["ex"][0:tsz, :], W["lgs"][0:tsz, :], AF.Exp, bias=W["ntp"][0:tsz, 0:1]))
        k.op("dve", [wl], [wl], lambda e: e.tensor_tensor(W["em"][0:tsz, :], W["ex"][0:tsz, :], W["mk"][0:tsz, :], ALU.mult))
        k.op("dve", [wl], [wl], lambda e: e.reduce_sum(W["dn"][0:tsz, :], W["em"][0:tsz, :], AX.X))
        k.op("dve", [wl], [wl], lambda e: e.reciprocal(W["dn"][0:tsz, :], W["dn"][0:tsz, :]))
        k.op("dve", [wl], [wl],
             lambda e: e.tensor_scalar(W["gt"][0:tsz, :], W["em"][0:tsz, :], W["dn"][0:tsz, 0:1], None, ALU.mult))
        ps3, ps3t = k.ps()
        k.op("pe", [wl, ident_tl], [ps3t],
             lambda e: e.transpose(ps3[0:8, 0:tsz], W["gt"][0:tsz, 0:8], ident[0:tsz, 0:tsz]))
        k.op("act", [ps3t], [GT_tl], lambda e: e.copy(GT[0:8, t0:t0 + tsz], ps3[0:8, 0:tsz]))
    return GT, GT_tl, sel, sel_tl


def phase_c(k, c, io, h, h_tl, kind, final):
    NE = 4 if kind == "dense" else 8
    with ExitStack() as es:
        wpool = WPool(k, es, 5)
        nt = NormTmp(k, es)
        g_ffn, g_ffn_tl = load_small(k, es, "g_ffn", io["ffn_g"], [128, 16])
        with ExitStack() as e1:
            g_ssm, g_ssm_tl = load_small(k, e1, "g_ssm", io["ssm_g"], [128, 8])
            mixed = k.sb(e1, "mixed", [128, 16, T], BF16)
            mixed_tl = [Tl("mixed") for _ in CH]
            for kt in range(8):
                k.dma("sp", mixed[:, kt, :], io["att_n"][kt * 128:(kt + 1) * 128, :], io["att_n_tl"], mixed_tl)
            gf = k.sb(e1, "gf", [128, 8, T], F32)
            gf_tl = [Tl("gf") for _ in CH]
            msk, msk_tl = load_small(k, e1, "msk", io["msk"], [128, 2])
            for kt in range(4):
                k.dma("sp", gf[:, kt, :], io["g_loc"][kt * 128:(kt + 1) * 128, :], io["g_loc_tl"], gf_tl)
            gpa = [(k.sb(e1, "gpa", [128, T], F32), Tl("gpa")) for _ in range(2)]
            gpb = [(k.sb(e1, "gpb", [128, T], F32), Tl("gpb")) for _ in range(2)]
            for kt in range(4):
                a_, atl = gpa[kt % 2]
                b_, btl = gpb[kt % 2]
                s0, s0tl = io["g_par"](0, kt)
                s1, s1tl = io["g_par"](1, kt)
                k.dma("sp", a_[:], s0, s0tl, [atl])
                k.dma("sp", b_[:], s1, s1tl, [btl])
                k.op("dve", [atl, msk_tl], [atl], lambda e: e.tensor_scalar(a_[:], a_[:], msk[:, 1:2], None, ALU.mult))
                k.op("dve", [atl, btl, msk_tl], gf_tl,
                     lambda e: e.scalar_tensor_tensor(gf[:, 4 + kt, :], b_[:], msk[:, 0:1], a_[:], ALU.mult, ALU.add))
            gb = k.sb(e1, "gb", [128, 8, T], BF16)
            gb_tl = [Tl("gb") for _ in CH]
            for ci, (n0, nsz) in enumerate(CH):
                for kt in range(8):
                    if kt % 2 == 0:
                        k.op("act", [gf_tl[ci]], [gb_tl[ci]], lambda e: e.copy(gb[:, kt, n0:n0 + nsz], gf[:, kt, n0:n0 + nsz]))
                    else:
                        k.op("dve", [gf_tl[ci]], [gb_tl[ci]], lambda e: e.tensor_copy(gb[:, kt, n0:n0 + nsz], gf[:, kt, n0:n0 + nsz]))
            sig = [(k.sb(e1, "sig", [128, 344], F32), Tl("sig")) for _ in range(2)]
            cnt = [0]

            def evac_glu(m, ci, ps, pst, n0, nsz):
                sg, sgl = sig[cnt[0] % 2]
                cnt[0] += 1
                k.op("act", [pst], [sgl], lambda e: e.activation(sg[:, 0:nsz], ps[:, 0:nsz], AF.Sigmoid))
                k.op("dve", [sgl, gf_tl[ci]], [gf_tl[ci]],
                     lambda e: e.tensor_tensor(gf[:, m, n0:n0 + nsz], gf[:, m, n0:n0 + nsz], sg[:, 0:nsz], ALU.mult))

            linear(k, wpool, io["w_glu_t"], 8, 8, 128,
                   lambda kt, ci: (gb[:, kt, CH[ci][0]:CH[ci][0] + CH[ci][1]], gb_tl[ci]), CH, evac_glu)
            rmsnorm(k, c, nt, gf, gf_tl, 8, CH, g_ssm, g_ssm_tl, mixed, mixed_tl, 1024, out_off=8)

            def evac_out(m, ci, ps, pst, n0, nsz):
                k.op("dve", [pst, h_tl[ci]], [h_tl[ci]],
                     lambda e: e.tensor_tensor(h[:, m, n0:n0 + nsz], h[:, m, n0:n0 + nsz], ps[:, 0:nsz], ALU.add))

            linear(k, wpool, io["w_out_t"], 16, 16, 128,
                   lambda kt, ci: (mixed[:, kt, CH[ci][0]:CH[ci][0] + CH[ci][1]], mixed_tl[ci]), CH, evac_out)
            k.barrier()
        with ExitStack() as e2:
            hn = k.sb(e2, "hn2", [128, 16, T], BF16)
            hn_tl = [Tl("hn2") for _ in CH]
            rsk = k.sb(e2, "rsk", [128, T], F32)
            rsk_tl = [Tl("rsk") for _ in CH]
            rmsnorm(k, c, nt, h, h_tl, 16, CH, g_ffn, g_ffn_tl, hn, hn_tl, D, keep=(rsk, rsk_tl))
            gbt = None
            if kind == "moe":
                gbt = moe_gates(k, c, e2, io, h, h_tl, g_ffn, g_ffn_tl, rsk, rsk_tl)
            hid = k.sb(e2, "hid", [128, 11, T], BF16)
            hid_tl = [Tl("hid") for _ in CH]
            sil = [(k.sb(e2, "sil", [128, 344], F32), Tl("sil")) for _ in range(2)]
            tt = [(k.sb(e2, "tt", [128, 344], F32), Tl("tt")) for _ in range(2)]
            gbc = [(k.sb(e2, "gbc", [128, T], F32), Tl("gbc")) for _ in range(2)] if kind == "moe" else None
            n_ev = [0]
            for ex in range(NE):
                gcur = None
                if kind == "moe":
                    gcur = gbc[ex % 2]
                    GT, GT_tl, sel, sel_tl = gbt
                    for ci, (n0, nsz) in enumerate(CH):
                        ps, pst = k.ps()
                        k.op("pe", [GT_tl, sel_tl], [pst],
                             lambda e: e.matmul(ps[:, 0:nsz], sel[0:8, ex, :], GT[0:8, n0:n0 + nsz], start=True, stop=True))
                        k.op("act", [pst], [gcur[1]], lambda e: e.copy(gcur[0][:, n0:n0 + nsz], ps[:, 0:nsz]))
                for m in range(11):
                    sg_, sgtl = wpool.get()
                    su_, sutl = wpool.get()
                    k.dma("pool", sg_[:, 0:16, :], io["wg_t"][ex, m], [], [sgtl])
                    k.dma("pool", su_[:, 0:16, :], io["wu_t"][ex, m], [], [sutl])
                    for ci, (n0, nsz) in enumerate(CH):
                        pg, pgt = k.ps()
                        pu, put = k.ps()
                        for kt in range(16):
                            k.op("pe", [sgtl, hn_tl[ci]], [pgt],
                                 lambda e: e.matmul(pg[:, 0:nsz], sg_[:, kt, :], hn[:, kt, n0:n0 + nsz],
                                                    start=(kt == 0), stop=(kt == 15)))
                        for kt in range(16):
                            k.op("pe", [sutl, hn_tl[ci]], [put],
                                 lambda e: e.matmul(pu[:, 0:nsz], su_[:, kt, :], hn[:, kt, n0:n0 + nsz],
                                                    start=(kt == 0), stop=(kt == 15)))
                        sl, sltl = sil[n_ev[0] % 2]
                        k.op("act", [pgt], [sltl], lambda e: e.activation(sl[:, 0:nsz], pg[:, 0:nsz], AF.Silu))
                        if kind == "dense":
                            k.op("dve", [put, sltl], [hid_tl[ci]],
                                 lambda e: e.tensor_tensor(hid[:, m, n0:n0 + nsz], pu[:, 0:nsz], sl[:, 0:nsz], ALU.mult))
                        else:
                            t_, ttl = tt[n_ev[0] % 2]
                            k.op("dve", [put, sltl], [ttl],
                                 lambda e: e.tensor_tensor(t_[:, 0:nsz], pu[:, 0:nsz], sl[:, 0:nsz], ALU.mult))
                            k.op("dve", [ttl, gcur[1]], [hid_tl[ci]],
                                 lambda e: e.tensor_tensor(hid[:, m, n0:n0 + nsz], t_[:, 0:nsz], gcur[0][:, n0:n0 + nsz], ALU.mult))
                        n_ev[0] += 1

                def evac_down(m, ci, ps, pst, n0, nsz):
                    k.op("dve", [pst, h_tl[ci]], [h_tl[ci]],
                         lambda e: e.tensor_tensor(h[:, m, n0:n0 + nsz], h[:, m, n0:n0 + nsz], ps[:, 0:nsz], ALU.add))

                linear(k, wpool, io["wd_t"][ex], 16, 11, 128,
                       lambda kt, ci: (hid[:, kt, CH[ci][0]:CH[ci][0] + CH[ci][1]], hid_tl[ci]), CH, evac_down)
            k.barrier()
        if final:
            with ExitStack() as e3:
                g_fin, g_fin_tl = load_small(k, e3, "g_fin", io["fin_g"], [128, 16])
                stg = [(k.sb(e3, "ostg", [128, 16, 344], F32), Tl("ostg")) for _ in range(1)]
                o, otl = stg[0]
                for ci, (n0, nsz) in enumerate(CH):
                    rmsnorm(k, c, nt, h[:, :, n0:n0 + nsz], [h_tl[ci]], 16, [(0, nsz)], g_fin, g_fin_tl, o, [otl], D)
                    for kt in range(16):
                        k.dma("sp", io["hT_out"][kt * 128:(kt + 1) * 128, n0:n0 + nsz], o[:, kt, 0:nsz], [otl], [])
                k.barrier()


RG_PAIRS = [[0, 1], [2, 3], [4, 5], [6, 7]]
X1R = 832


def fused_inputs():
    f, L = "f", DEPTH
    ins = {"hT": ([D, T], f), "ropeC": ([64, T], f), "ropeS": ([64, T], f), "iota_t": ([128, LTOT], "i"),
           "ident": ([128, 128], f), "sel": ([8, 8, 128], f), "fin_g": ([128, 16], f),
           "w_in_t": ([L, 15, 128, 16, 128], f), "mix_g": ([L, 128, 16], f), "q_g": ([L, 128, 4], f),
           "kv_g": ([L, 128, 2], f), "w_uq_t": ([L, 16, 128, 4, 128], f), "w_uk_t": ([L, 8, 128, 2, 128], f),
           "w_v": ([L, 128, 2, 1024], f), "att_g": ([L, 128, 8], f),
           "s5_lre_s": ([L, 128, NGD], f), "s5_lim_s": ([L, 128, NGD], f), "s5_ls_s": ([L, 128, NGD], f),
           "s5_lre_b": ([L, 128, NSL * 64], f), "s5_lim_b": ([L, 128, NSL * 64], f), "s5_ls_b": ([L, 128, NSL * 64], f),
           "s5_bre": ([L, 128, NSL * 64], f), "s5_bim": ([L, 128, NSL * 64], f),
           "s5_cta": ([L, 128, NGD, 16], f), "s5_ctb": ([L, 128, NGD, 16], f), "s5_d": ([L, 128, NST], f),
           "ssm_g": ([L, 128, 8], f), "ffn_g": ([L, 128, 16], f),
           "w_glu_t": ([L, 8, 128, 8, 128], f), "w_out_t": ([L, 16, 128, 16, 128], f),
           "wg_d": ([2, 4, 11, 128, 16, 128], f), "wu_d": ([2, 4, 11, 128, 16, 128], f), "wd_d": ([2, 4, 16, 128, 11, 128], f),
           "wg_m": ([2, 8, 11, 128, 16, 128], f), "wu_m": ([2, 8, 11, 128, 16, 128], f), "wd_m": ([2, 8, 16, 128, 11, 128], f),
           "router": ([2, 128, 16, 8], f), "msk": ([128, 2], f)}
    return ins


class FusedCtx:
    pass


def load_h(k, es, io):
    h = k.sb(es, "h", [128, 16, T], F32)
    h_tl = [Tl("h") for _ in CH]
    for kt in range(16):
        k.dma("sp", h[:, kt, :], io["hT"][kt * 128:(kt + 1) * 128, :], [], h_tl)
    return h, h_tl


def _dt(name):
    return {"f": F32, "b": BF16, "i": I16}[name]


def build_fused(nlayers=DEPTH):
    nc = bass.Bass("TRN2", target_bir_lowering=False)
    ins = fused_inputs()
    io = {}
    for nm, (shape, dt) in ins.items():
        io[nm] = nc.dram_tensor(nm, shape, _dt(dt), kind="ExternalInput").ap()
    io["hT_out"] = nc.dram_tensor("hT_out", [D, T], F32, kind="ExternalOutput").ap()
    x1_in = nc.dram_tensor("x1_in", [X1R, T], BF16)
    x1_out = nc.dram_tensor("x1_out", [2 * X1R, T], BF16)
    x2_in = [nc.dram_tensor("x2_in%d" % i, [256, T], F32) for i in range(2)]
    x2_out = [nc.dram_tensor("x2_out%d" % i, [512, T], F32) for i in range(2)]
    cq_s = nc.dram_tensor("cq_s", [512, T], BF16)
    att_s = nc.dram_tensor("att_s", [1024, T], BF16)
    u_loc = nc.dram_tensor("u_loc", [512, T], BF16)
    g_loc = nc.dram_tensor("g_loc", [512, T], F32)
    tl = {n: Tl(n) for n in ("x1_in", "x1_out", "x2_in0", "x2_in1", "x2_out0", "x2_out1", "cq", "att", "uloc", "gloc")}
    with ExitStack() as es:
        k = K(nc, es)
        c = make_consts(k, es)
        h, h_tl = load_h(k, es, io)
        for l in range(nlayers):
            kind = "dense" if l % 2 == 0 else "moe"
            final = (l == DEPTH - 1)
            ioa = {"mix_g": io["mix_g"][l], "q_g": io["q_g"][l], "kv_g": io["kv_g"][l], "ropeC": io["ropeC"],
                   "ropeS": io["ropeS"], "w_in_t": io["w_in_t"][l], "cq_n": cq_s.ap(), "cq_n_tl": [tl["cq"]],
                   "kvx": x1_in.ap(), "kvx_tl": [tl["x1_in"]]}

            def u_dst(m):
                if m < 4:
                    return u_loc.ap()[m * 128:(m + 1) * 128, :], [tl["uloc"]]
                return x1_in.ap()[320 + (m - 4) * 128:320 + (m - 3) * 128, :], [tl["x1_in"]]
            ioa["u_dst"] = u_dst
            phase_a(k, c, ioa, h, h_tl)
            k.collective(x1_in, x1_out, [tl["x1_in"]], [tl["x1_out"]], RG_PAIRS)
            iob = {"cq_n": cq_s.ap(), "cq_n_tl": [tl["cq"]], "att_g": io["att_g"][l], "ropeC": io["ropeC"],
                   "ropeS": io["ropeS"], "w_v": io["w_v"][l], "w_uq_t": io["w_uq_t"][l], "w_uk_t": io["w_uk_t"][l],
                   "att_n": att_s.ap(), "att_n_tl": [tl["att"]], "iota_t": io["iota_t"], "msk": io["msk"],
                   "u_loc": u_loc.ap(), "u_loc_tl": [tl["uloc"]]}
            for nm in ("s5_lre_s", "s5_lim_s", "s5_ls_s", "s5_lre_b", "s5_lim_b", "s5_ls_b", "s5_bre", "s5_bim",
                       "s5_cta", "s5_ctb", "s5_d"):
                iob[nm] = io[nm][l]
            iob["kvx_half"] = lambda hh: (x1_out.ap()[hh * X1R:hh * X1R + 320, :], [tl["x1_out"]])
            iob["u_par"] = lambda hh: (x1_out.ap()[hh * X1R + 320:(hh + 1) * X1R, :], [tl["x1_out"]])

            def g_dst(which, gl):
                if which == 0:
                    return g_loc.ap()[gl * 16:(gl + 1) * 16, :], [tl["gloc"]]
                i2 = gl // 16
                r0 = (gl % 16) * 16
                return x2_in[i2].ap()[r0:r0 + 16, :], [tl["x2_in%d" % i2]]
            iob["g_dst"] = g_dst
            phase_attn(k, c, iob)
            phase_s5(k, c, iob)
            for i2 in range(2):
                k.collective(x2_in[i2], x2_out[i2], [tl["x2_in%d" % i2]], [tl["x2_out%d" % i2]], RG_PAIRS)
            ioc = {"att_n": att_s.ap(), "att_n_tl": [tl["att"]], "ssm_g": io["ssm_g"][l], "ffn_g": io["ffn_g"][l],
                   "w_glu_t": io["w_glu_t"][l], "w_out_t": io["w_out_t"][l], "msk": io["msk"],
                   "g_loc": g_loc.ap(), "g_loc_tl": [tl["gloc"]], "hT_out": io["hT_out"], "fin_g": io["fin_g"],
                   "ident": io["ident"], "sel": io["sel"]}
            sfx = "d" if kind == "dense" else "m"
            ioc["wg_t"], ioc["wu_t"], ioc["wd_t"] = io["wg_" + sfx][l // 2], io["wu_" + sfx][l // 2], io["wd_" + sfx][l // 2]
            if kind == "moe":
                ioc["router"] = io["router"][l // 2]
            ioc["g_par"] = lambda blk, kt: (x2_out[kt // 2].ap()[blk * 256 + (kt % 2) * 128:blk * 256 + (kt % 2) * 128 + 128, :],
                                            [tl["x2_out%d" % (kt // 2)]])
            phase_c(k, c, ioc, h, h_tl, kind, final or (l == nlayers - 1 and nlayers < DEPTH))
        k.finish()
    return nc


def tile_w(W, mw=128):
    Kd, Md = W.shape
    KT, MT = Kd // 128, Md // mw
    return np.ascontiguousarray(W.reshape(KT, 128, MT, mw).transpose(2, 1, 0, 3))


def col_gain(g):
    n = g.shape[0] // 128
    return np.ascontiguousarray(g.reshape(n, 128).T)


def rope_consts():
    inv = (10000.0 ** (-np.arange(0, 64, 2, dtype=np.float32) / 64)).astype(np.float32)
    ang = np.arange(LTOT, dtype=np.float32)[:, None] * inv[None, :]
    cos, sin = np.cos(ang).astype(np.float32).T, np.sin(ang).astype(np.float32).T
    C = np.concatenate([cos, cos], 0)
    S = np.concatenate([-sin, sin], 0)
    return np.ascontiguousarray(C), np.ascontiguousarray(S)


def swap_halves(Wc):
    return np.concatenate([Wc[:, 32:64], Wc[:, 0:32]], axis=1)


def chan_perm(hf):
    own = np.arange(hf * 512, (hf + 1) * 512)
    par = np.arange((1 - hf) * 512, (2 - hf) * 512)
    return np.concatenate([own, par])


def prep_shared(inp):
    P = {}
    L = DEPTH
    uq, uk, wv = [], [], []
    for l in range(L):
        wq = inp["w_uq"][l]
        cols = []
        for hd in range(8):
            b = hd * 192
            r = wq[:, b + 128:b + 192]
            cols += [wq[:, b:b + 128], r, swap_halves(r)]
        uq.append(tile_w(np.concatenate(cols, axis=1)))
        wkv = inp["w_ukv"][l]
        uk.append(tile_w(np.concatenate([wkv[:, hd * 256:hd * 256 + 128] for hd in range(8)], axis=1)))
        v = np.concatenate([wkv[:, hd * 256 + 128:hd * 256 + 256] for hd in range(8)], axis=1)
        wv.append(np.ascontiguousarray(v.reshape(2, 128, 1024).transpose(1, 0, 2)))
    P["w_uq_t"], P["w_uk_t"], P["w_v"] = np.stack(uq), np.stack(uk), np.stack(wv)
    for nm, key in (("mix_g", "mix_norm"), ("q_g", "q_norm"), ("kv_g", "kv_norm"), ("att_g", "attn_out_norm"),
                    ("ffn_g", "ffn_norm")):
        P[nm] = np.stack([col_gain(inp[key][l]) for l in range(L)])
    P["fin_g"] = col_gain(inp["final_norm"])
    for sfx, g, u, d, ne in (("d", "dense_w_gate", "dense_w_up", "dense_w_down", 4), ("m", "moe_w_gate", "moe_w_up", "moe_w_down", 8)):
        wg, wu, wd = [], [], []
        for i in range(2):
            if sfx == "d":
                G, U, Dn = inp[g][i], inp[u][i], inp[d][i]
                wg.append(np.stack([tile_w(G[:, e * 1408:(e + 1) * 1408]) for e in range(4)]))
                wu.append(np.stack([tile_w(U[:, e * 1408:(e + 1) * 1408]) for e in range(4)]))
                wd.append(np.stack([tile_w(Dn[e * 1408:(e + 1) * 1408, :]) for e in range(4)]))
            else:
                wg.append(np.stack([tile_w(inp[g][i][e]) for e in range(8)]))
                wu.append(np.stack([tile_w(inp[u][i][e]) for e in range(8)]))
                wd.append(np.stack([tile_w(inp[d][i][e]) for e in range(8)]))
        P["wg_" + sfx], P["wu_" + sfx], P["wd_" + sfx] = np.stack(wg), np.stack(wu), np.stack(wd)
    P["router"] = np.stack([np.ascontiguousarray(inp["moe_router"][i].reshape(16, 128, 8).transpose(1, 0, 2)) for i in range(2)])
    return P


def prep_half(inp, hf):
    P = {}
    perm = chan_perm(hf)
    win, wglu, wout, ssmg = [], [], [], []
    for l in range(DEPTH):
        w_in = inp["w_in"][l]
        kr = w_in[:, 768:832]
        w_in_x = np.concatenate([w_in[:, 0:768], kr, swap_halves(kr), w_in[:, 832:][:, perm]], axis=1)
        win.append(tile_w(w_in_x))
        wglu.append(tile_w(inp["ssm_w_glu"][l][perm][:, perm]))
        wo = inp["w_out"][l]
        wout.append(tile_w(np.concatenate([wo[0:1024], wo[1024:][perm]], axis=0)))
        ssmg.append(col_gain(inp["ssm_out_norm"][l][perm]))
    P["w_in_t"], P["w_glu_t"], P["w_out_t"], P["ssm_g"] = np.stack(win), np.stack(wglu), np.stack(wout), np.stack(ssmg)
    s5 = [prep_s5(inp, l, hf) for l in range(DEPTH)]
    for nm in s5[0]:
        P[nm] = np.stack([s5[l][nm] for l in range(DEPTH)])
    m = np.zeros((128, 2), np.float32)
    m[:, hf] = 1.0
    P["msk"] = m
    return P


def prep_s5(inp, l, hf):
    G0 = hf * NG
    lre = inp["ssm_lambda_re"][l][:, G0:G0 + NG]
    lim = inp["ssm_lambda_im"][l][:, G0:G0 + NG]
    ls = inp["ssm_log_step"][l][:, G0:G0 + NG]
    bre = inp["ssm_b_re"][l][:, G0:G0 + NG]
    bim = inp["ssm_b_im"][l][:, G0:G0 + NG]
    cre = inp["ssm_c_re"][l][:, G0:G0 + NG]
    cim = inp["ssm_c_im"][l][:, G0:G0 + NG]
    dsk = inp["ssm_d"][l][G0 * 16:(G0 + NG) * 16]
    S = {}
    st = lambda a: np.ascontiguousarray(np.concatenate([a.reshape(NGD, 64).T] * 2, axis=0))
    S["s5_lre_s"] = st(lre)
    S["s5_lim_s"] = st(lim)
    S["s5_ls_s"] = np.ascontiguousarray(np.broadcast_to(ls.reshape(1, NGD), (128, NGD)))

    def pad_rows(a, bcast):
        out = np.zeros((4, 32, NST, 2, 64), np.float32)
        for q4 in range(4):
            for s in range(NST):
                g = min(3 * s + min(q4, 2), NG - 1)
                for d in range(2):
                    if bcast:
                        out[q4, :, s, d, :] = a[d, g][None, :]
                    elif q4 < 3:
                        out[q4, 0:16, s, d, :] = a[d, g].T
        return np.ascontiguousarray(out.reshape(128, NSL * 64))
    S["s5_lre_b"] = pad_rows(lre, True)
    S["s5_lim_b"] = pad_rows(lim, True)
    S["s5_ls_b"] = pad_rows(np.broadcast_to(ls[:, :, None], (2, NG, 64)), True)
    S["s5_bre"] = pad_rows(bre, False)
    S["s5_bim"] = pad_rows(bim, False)
    crt = cre.reshape(NGD, 16, 64).transpose(2, 0, 1)
    cit = cim.reshape(NGD, 16, 64).transpose(2, 0, 1)
    S["s5_cta"] = np.ascontiguousarray(np.concatenate([crt, cit], axis=0))
    S["s5_ctb"] = np.ascontiguousarray(np.concatenate([cit, crt], axis=0))
    dp = np.zeros((4, 32, NST), np.float32)
    for q4 in range(3):
        for s in range(NST):
            g = 3 * s + q4
            if g < NG:
                dp[q4, 0:16, s] = dsk[g * 16:(g + 1) * 16]
    S["s5_d"] = np.ascontiguousarray(dp.reshape(128, NST))
    return S


_PROG = {}


def make_in_maps(inp):
    x = inp["x"]
    meta = inp["meta_tokens"]
    ropeC, ropeS = rope_consts()
    iota_t = np.ascontiguousarray(np.broadcast_to(np.arange(LTOT, dtype=np.int16)[None, :], (128, LTOT)))
    ident = np.eye(128, dtype=np.float32)
    sel = np.zeros((8, 8, 128), np.float32)
    for e in range(8):
        sel[e, e, :] = 1.0
    shared = prep_shared(inp)
    halves = [prep_half(inp, 0), prep_half(inp, 1)]
    maps = []
    for cidx in range(NCORES):
        b, hf = cidx // 2, cidx % 2
        full = np.concatenate([meta, x[b]], axis=0)
        m = {"hT": np.ascontiguousarray(full[hf * T:(hf + 1) * T].T),
             "ropeC": np.ascontiguousarray(ropeC[:, hf * T:(hf + 1) * T]),
             "ropeS": np.ascontiguousarray(ropeS[:, hf * T:(hf + 1) * T]),
             "iota_t": iota_t, "ident": ident, "sel": sel}
        m.update(shared)
        m.update(halves[hf])
        maps.append(m)
    return maps


def kernel(**inp):
    inp = {k_: np.asarray(v) for k_, v in inp.items()}
    B = inp["x"].shape[0]
    maps = make_in_maps(inp)
    if "fused" not in _PROG:
        _PROG["fused"] = build_fused()
    res = run_bass_kernel_spmd(_PROG["fused"], maps, core_ids=list(range(NCORES)))
    hT = [res.results[c_]["hT_out"] for c_ in range(NCORES)]
    out = np.empty((B, SEQ, D), np.float32)
    for b in range(B):
        full = np.concatenate([hT[2 * b].T, hT[2 * b + 1].T], axis=0)
        out[b] = full[NMETA:]
    return out
```
